# Optimizing a Trainium2 kernel written in Bass

```python
import math
import jax
import jax.numpy as jnp
from jax import lax
import numpy as np

D_MODEL = 2048
BATCH = 2
SEQ = 16384
DEPTH = 1

DA_HEADS = 8
DA_QK_DIM = 64
DA_V_DIM = 2 * DA_QK_DIM
GDN_HEADS = 8
GDN_DK = 128
GDN_DV = 128
GDN_CONV = 5
GDN_CHUNK = 64
Q_BLOCK = 128
REL_BUCKETS = 32
REL_MAX_DIST = 128
MEM_LEN = 256
CA_HEADS = 4
CA_HEAD_DIM = 128
N_EXPERTS = 16
EXPERT_FF = 2048
CAPACITY = 2
NORM_EPS = 1e-6

DA_QK_COLS = DA_HEADS * 2 * DA_QK_DIM
DA_V_COLS = DA_HEADS * DA_V_DIM
GDN_QK_COLS = GDN_HEADS * GDN_DK
GDN_V_COLS = GDN_HEADS * GDN_DV
MIX_WIDTH = DA_V_COLS + GDN_V_COLS
IN_SPLIT_SIZES = (DA_QK_COLS, DA_QK_COLS, DA_V_COLS,
                  GDN_QK_COLS, GDN_QK_COLS, GDN_V_COLS, GDN_V_COLS,
                  GDN_HEADS, GDN_HEADS, GDN_HEADS, GDN_HEADS)
IN_COLS = sum(IN_SPLIT_SIZES)

kernel_name = 'hybrid_diffattn_gdn_ec_moe_encoder'


def rms_norm(x, w, eps=NORM_EPS):
    xf = x.astype(jnp.float32)
    y = xf * lax.rsqrt(jnp.mean(xf * xf, axis=-1, keepdims=True) + eps)
    return (y * w.astype(jnp.float32)).astype(x.dtype)


def l2_normalize(t, eps=1e-6):
    return t * lax.rsqrt(jnp.sum(t * t, axis=-1, keepdims=True) + eps)


def t5_bucket(rel):
    half = REL_BUCKETS // 2
    max_exact = half // 2
    n = jnp.abs(rel)
    large = max_exact + (jnp.log(jnp.maximum(n, 1).astype(jnp.float32) / max_exact)
                         / math.log(REL_MAX_DIST / max_exact) * (half - max_exact)).astype(jnp.int32)
    large = jnp.minimum(large, half - 1)
    return jnp.where(rel > 0, half, 0) + jnp.where(n < max_exact, n, large)


def short_conv(u, w):
    k = w.shape[0]
    return lax.conv_general_dilated(u, w[:, None, :].astype(u.dtype), window_strides=(1,),
                                    padding=[((k - 1) // 2, k // 2)],
                                    dimension_numbers=('NWC', 'WIO', 'NWC'),
                                    feature_group_count=u.shape[-1])


def diff_attention(q1, q2, k1, k2, v, lam, bias_table):
    b, h, s, dh = q1.shape
    nb = s // Q_BLOCK
    scale = dh ** -0.5
    kpos = jnp.arange(s, dtype=jnp.int32)
    table = bias_table.astype(jnp.float32)

    def to_blocks(t):
        return jnp.moveaxis(t.reshape(b, h, nb, Q_BLOCK, dh), 2, 0)

    def one_block(args):
        qb1, qb2, q0 = args
        qpos = q0 + jnp.arange(Q_BLOCK, dtype=jnp.int32)
        bias = jnp.transpose(table[t5_bucket(kpos[None, :] - qpos[:, None])], (2, 0, 1))
        s1 = jnp.einsum('bhqd,bhkd->bhqk', qb1, k1).astype(jnp.float32) * scale + bias
        s2 = jnp.einsum('bhqd,bhkd->bhqk', qb2, k2).astype(jnp.float32) * scale + bias
        p = jax.nn.softmax(s1, axis=-1) - lam * jax.nn.softmax(s2, axis=-1)
        return jnp.einsum('bhqk,bhkd->bhqd', p.astype(v.dtype), v)

    starts = jnp.arange(nb, dtype=jnp.int32) * Q_BLOCK
    o = lax.map(one_block, (to_blocks(q1), to_blocks(q2), starts))
    return jnp.moveaxis(o, 0, 2).reshape(b, h, s, v.shape[-1])


def gated_delta_chunked(q, k, v, g, beta):
    b, h, s, dk = q.shape
    dv = v.shape[-1]
    c = GDN_CHUNK
    n = s // c
    q = (q * dk ** -0.5).reshape(b, h, n, c, dk)
    k = k.reshape(b, h, n, c, dk)
    v = v.reshape(b, h, n, c, dv)
    beta = beta.reshape(b, h, n, c, 1)
    gc = jnp.cumsum(g.reshape(b, h, n, c), axis=-1)
    incl = jnp.tril(jnp.ones((c, c), dtype=bool))
    strict = jnp.tril(jnp.ones((c, c), dtype=bool), -1)
    decay = jnp.where(incl, jnp.exp(jnp.where(incl, gc[..., :, None] - gc[..., None, :], 0.0)), 0.0)
    kb = k * beta
    lower = jnp.where(strict, jnp.einsum('bhnid,bhnjd->bhnij', kb, k) * decay, 0.0)
    wy = lower + jnp.eye(c, dtype=lower.dtype)
    u = lax.linalg.triangular_solve(wy, v * beta, left_side=True, lower=True, unit_diagonal=True)
    w = lax.linalg.triangular_solve(wy, kb * jnp.exp(gc)[..., None], left_side=True, lower=True,
                                    unit_diagonal=True)
    intra = jnp.where(incl, jnp.einsum('bhnid,bhnjd->bhnij', q, k) * decay, 0.0)
    q_dec = q * jnp.exp(gc)[..., None]
    k_tail = k * jnp.exp(gc[..., -1:] - gc)[..., None]
    chunk_decay = jnp.exp(gc[..., -1])

    def step(state, xs):
        u_i, w_i, qd_i, kt_i, a_i, cd_i = xs
        v_new = u_i - jnp.einsum('bhck,bhkv->bhcv', w_i, state)
        o_i = jnp.einsum('bhck,bhkv->bhcv', qd_i, state) + jnp.einsum('bhij,bhjv->bhiv', a_i, v_new)
        state = state * cd_i[..., None, None] + jnp.einsum('bhck,bhcv->bhkv', kt_i, v_new)
        return state, o_i

    xs = tuple(jnp.moveaxis(t, 2, 0) for t in (u, w, q_dec, k_tail, intra, chunk_decay))
    _, o = lax.scan(step, jnp.zeros((b, h, dk, dv), jnp.float32), xs)
    return jnp.moveaxis(o, 0, 2).reshape(b, h, s, dv)


def memory_cross_attention(hx, hm, w_cq, w_ckv, w_co):
    b, s, _ = hx.shape
    q = jnp.einsum('bsd,de->bse', hx, w_cq).reshape(b, s, CA_HEADS, CA_HEAD_DIM)
    kv = jnp.einsum('bmd,de->bme', hm, w_ckv).reshape(b, hm.shape[1], 2, CA_HEADS, CA_HEAD_DIM)
    sc = jnp.einsum('bshd,bmhd->bhsm', q, kv[:, :, 0]).astype(jnp.float32) * CA_HEAD_DIM ** -0.5
    p = jax.nn.softmax(sc, axis=-1).astype(hx.dtype)
    o = jnp.einsum('bhsm,bmhd->bshd', p, kv[:, :, 1]).reshape(b, s, CA_HEADS * CA_HEAD_DIM)
    return jnp.einsum('bse,ed->bsd', o, w_co)


def expert_choice_moe(h, w_router, w_gate, w_up, w_down):
    b, s, d = h.shape
    cap = CAPACITY * s // N_EXPERTS
    aff = jax.nn.softmax(jnp.einsum('bsd,de->bse', h, w_router).astype(jnp.float32), axis=-1)
    gate, idx = lax.top_k(jnp.swapaxes(aff, 1, 2), cap)
    xs = jax.vmap(lambda hb, ib: hb[ib])(h, idx)
    a = jnp.einsum('becd,edf->becf', xs, w_gate)
    up = jnp.einsum('becd,edf->becf', xs, w_up)
    y = jnp.einsum('becf,efd->becd', jax.nn.silu(a) * up, w_down) * gate[..., None].astype(h.dtype)
    return jax.vmap(lambda ib, yb: jnp.zeros((s, d), yb.dtype).at[ib.reshape(-1)].add(yb.reshape(-1, d)))(idx, y)


def setup_inputs(seed: int = 0) -> dict:
    key = jax.random.key(seed)
    ks = jax.random.split(key, 32)
    f32 = jnp.float32
    L = DEPTH

    def nrm(k, shape, scale):
        return jax.random.normal(k, shape, f32) * scale

    def gain(k, shape):
        return 1.0 + 0.02 * jax.random.normal(k, shape, f32)

    a_init = jax.random.uniform(ks[12], (L, 2, GDN_HEADS), f32, 1.0, 16.0)
    dt = jnp.exp(jax.random.uniform(ks[13], (L, 2, GDN_HEADS), f32, math.log(1e-3), math.log(1e-1)))
    return {
        'x': nrm(ks[0], (BATCH, SEQ, D_MODEL), 1.0),
        'mem': nrm(ks[1], (BATCH, MEM_LEN, D_MODEL), 1.0),
        'rel_bias_table': nrm(ks[2], (REL_BUCKETS, DA_HEADS), 0.5),
        'norm_mix': gain(ks[3], (L, D_MODEL)),
        'w_in': nrm(ks[4], (L, D_MODEL, IN_COLS), D_MODEL ** -0.5),
        'conv_w': nrm(ks[5], (L, GDN_CONV, 2 * GDN_QK_COLS + GDN_V_COLS), GDN_CONV ** -0.5),
        'lambda_q1': nrm(ks[6], (L, DA_QK_DIM), 0.1),
        'lambda_k1': nrm(ks[7], (L, DA_QK_DIM), 0.1),
        'lambda_q2': nrm(ks[8], (L, DA_QK_DIM), 0.1),
        'lambda_k2': nrm(ks[9], (L, DA_QK_DIM), 0.1),
        'da_subln': gain(ks[10], (L, DA_V_DIM)),
        'gdn_a_log': jnp.log(a_init),
        'gdn_dt_bias': dt + jnp.log(-jnp.expm1(-dt)),
        'gdn_norm': gain(ks[11], (L, GDN_DV)),
        'w_out': nrm(ks[14], (L, MIX_WIDTH, D_MODEL), MIX_WIDTH ** -0.5),
        'norm_cross': gain(ks[15], (L, D_MODEL)),
        'norm_mem': gain(ks[16], (L, D_MODEL)),
        'w_cq': nrm(ks[17], (L, D_MODEL, CA_HEADS * CA_HEAD_DIM), D_MODEL ** -0.5),
        'w_ckv': nrm(ks[18], (L, D_MODEL, 2 * CA_HEADS * CA_HEAD_DIM), D_MODEL ** -0.5),
        'w_co': nrm(ks[19], (L, CA_HEADS * CA_HEAD_DIM, D_MODEL), (CA_HEADS * CA_HEAD_DIM) ** -0.5),
        'norm_moe': gain(ks[20], (L, D_MODEL)),
        'w_router': nrm(ks[21], (L, D_MODEL, N_EXPERTS), D_MODEL ** -0.5),
        'w_gate': nrm(ks[22], (L, N_EXPERTS, D_MODEL, EXPERT_FF), D_MODEL ** -0.5),
        'w_up': nrm(ks[23], (L, N_EXPERTS, D_MODEL, EXPERT_FF), D_MODEL ** -0.5),
        'w_down': nrm(ks[24], (L, N_EXPERTS, EXPERT_FF, D_MODEL), EXPERT_FF ** -0.5),
        'norm_final': gain(ks[25], (D_MODEL,)),
    }


def reference(x, mem, rel_bias_table, norm_mix, w_in, conv_w, lambda_q1, lambda_k1, lambda_q2, lambda_k2,
              da_subln, gdn_a_log, gdn_dt_bias, gdn_norm, w_out, norm_cross, norm_mem, w_cq, w_ckv, w_co,
              norm_moe, w_router, w_gate, w_up, w_down, norm_final):
    f32 = jnp.float32
    b, s, _ = x.shape
    split_at = np.cumsum(IN_SPLIT_SIZES)[:-1].tolist()

    def to_heads(t, n_heads, dim):
        return jnp.transpose(t.reshape(b, s, n_heads, dim), (0, 2, 1, 3))

    for l in range(DEPTH):
        lam_init = 0.8 - 0.6 * math.exp(-0.3 * l)
        h = rms_norm(x, norm_mix[l])
        proj = jnp.einsum('bsd,de->bse', h, w_in[l])
        da_q, da_k, da_v, g_q, g_k, g_v, g_z, a_f, b_f, a_b, b_b = jnp.split(proj, split_at, axis=-1)

        dq = to_heads(da_q, DA_HEADS, 2 * DA_QK_DIM)
        dk = to_heads(da_k, DA_HEADS, 2 * DA_QK_DIM)
        lam = (jnp.exp(jnp.sum(lambda_q1[l].astype(f32) * lambda_k1[l].astype(f32)))
               - jnp.exp(jnp.sum(lambda_q2[l].astype(f32) * lambda_k2[l].astype(f32))) + lam_init)
        o_da = diff_attention(dq[..., :DA_QK_DIM], dq[..., DA_QK_DIM:], dk[..., :DA_QK_DIM], dk[..., DA_QK_DIM:],
                              to_heads(da_v, DA_HEADS, DA_V_DIM), lam, rel_bias_table)
        o_da = rms_norm(jnp.transpose(o_da, (0, 2, 1, 3)), da_subln[l]) * (1.0 - lam_init)
        o_da = o_da.reshape(b, s, DA_V_COLS)

        qkv = jax.nn.silu(short_conv(jnp.concatenate([g_q, g_k, g_v], axis=-1), conv_w[l]))
        c_q, c_k, c_v = jnp.split(qkv, [GDN_QK_COLS, 2 * GDN_QK_COLS], axis=-1)
        gq = l2_normalize(to_heads(c_q, GDN_HEADS, GDN_DK).astype(f32))
        gk = l2_normalize(to_heads(c_k, GDN_HEADS, GDN_DK).astype(f32))
        gv = to_heads(c_v, GDN_HEADS, GDN_DV).astype(f32)
        log_a_f = jnp.swapaxes(-jnp.exp(gdn_a_log[l, 0].astype(f32))
                               * jax.nn.softplus(a_f.astype(f32) + gdn_dt_bias[l, 0].astype(f32)), 1, 2)
        log_a_b = jnp.swapaxes(-jnp.exp(gdn_a_log[l, 1].astype(f32))
                               * jax.nn.softplus(a_b.astype(f32) + gdn_dt_bias[l, 1].astype(f32)), 1, 2)
        beta_f = jnp.swapaxes(jax.nn.sigmoid(b_f.astype(f32)), 1, 2)
        beta_b = jnp.swapaxes(jax.nn.sigmoid(b_b.astype(f32)), 1, 2)
        o_fwd = gated_delta_chunked(gq, gk, gv, log_a_f, beta_f)
        o_bwd = jnp.flip(gated_delta_chunked(jnp.flip(gq, 2), jnp.flip(gk, 2), jnp.flip(gv, 2),
                                             jnp.flip(log_a_b, 2), jnp.flip(beta_b, 2)), 2)
        o_gdn = jnp.transpose(o_fwd + o_bwd, (0, 2, 1, 3))
        z = g_z.reshape(b, s, GDN_HEADS, GDN_DV).astype(f32)
        o_gdn = (rms_norm(o_gdn, gdn_norm[l]) * jax.nn.silu(z)).astype(x.dtype).reshape(b, s, GDN_V_COLS)

        x = x + jnp.einsum('bsm,md->bsd', jnp.concatenate([o_da, o_gdn], axis=-1), w_out[l])

        x = x + memory_cross_attention(rms_norm(x, norm_cross[l]), rms_norm(mem, norm_mem[l]),
                                       w_cq[l], w_ckv[l], w_co[l])

        x = x + expert_choice_moe(rms_norm(x, norm_moe[l]), w_router[l], w_gate[l], w_up[l], w_down[l])

    return rms_norm(x, norm_final)
```

```python
import os
import numpy as np
import ml_dtypes
from contextlib import ExitStack
import concourse.bass as bass
import concourse.mybir as mybir
from concourse.bass_utils import run_bass_kernel_spmd


F32 = mybir.dt.float32
BF16 = mybir.dt.bfloat16
I32 = mybir.dt.int32
AF = mybir.ActivationFunctionType
ALU = mybir.AluOpType
AX = mybir.AxisListType


class Buf:
    __slots__ = ("t", "_w", "_r", "dsem", "dcnt", "dsem2", "dcnt2", "name", "root", "excl")

    def __init__(self, t, name="", root=None, excl=False):
        self.excl = excl
        self.t = t
        self._w = None
        self._r = []
        self.dsem = None
        self.dcnt = 0
        self.dsem2 = None
        self.dcnt2 = 0
        self.name = name
        self.root = root

    @property
    def w(self):
        return (self.root or self)._w

    @w.setter
    def w(self, v):
        (self.root or self)._w = v

    @property
    def r(self):
        return (self.root or self)._r

    @r.setter
    def r(self, v):
        (self.root or self)._r = v

    def __getitem__(self, k):
        return self.t[k]


class View:
    def __init__(self, t, off, width):
        self.t, self.off, self.width = t, off, width

    def __getitem__(self, k):
        rows, cols = k
        a = 0 if cols.start is None else cols.start
        b = self.width if cols.stop is None else cols.stop
        return self.t[rows, self.off + a:self.off + b]


class Sched:
    ENG = ("pe", "dve", "act", "pool", "sp")

    def __init__(self, nc, stack):
        self.nc = nc
        self.stack = stack
        self.gstack = stack
        self.dsems = {}
        self.E = dict(pe=nc.tensor, dve=nc.vector, act=nc.scalar, pool=nc.gpsimd, sp=nc.sync)
        self.sem = {k: stack.enter_context(nc.semaphore("es_" + k)) for k in self.ENG}
        self.cnt = {k: 0 for k in self.ENG}
        self.seen = {}
        self.nsem = 5
        self.ninst = 0

    def sb(self, name, shape, dt):
        self.nsem += 0
        self._uid = getattr(self, "_uid", 0) + 1
        return Buf(self.stack.enter_context(self.nc.sbuf_tensor(f"s{self._uid}_{name}", shape, dt)), name)

    def ps(self, name, shape, dt=F32):
        self._uid = getattr(self, "_uid", 0) + 1
        return Buf(self.stack.enter_context(self.nc.psum_tensor(f"p{self._uid}_{name}", shape, dt)), name, excl=True)

    def ps_multi(self, name, n, width=128, dt=F32):
        self._uid = getattr(self, "_uid", 0) + 1
        t = self.stack.enter_context(self.nc.psum_tensor(f"p{self._uid}_{name}", [128, n * width], dt))
        root = Buf(t, name, excl=True)
        return [Buf(View(t, i * width, width), f"{name}{i}", root=root, excl=True) for i in range(n)]

    def newsem(self, name):
        self.nsem += 1
        self._uid = getattr(self, "_uid", 0) + 1
        return self.gstack.enter_context(self.nc.semaphore(f"m{self._uid}_{name}"))

    def _wait(self, eng, tk):
        if tk is None:
            return
        sem, val, peng = tk
        key = (eng, id(sem))
        if self.seen.get(key, 0) >= val:
            return
        self.seen[key] = val
        self.E[eng].wait_ge(sem, val)
        self.ninst += 1

    def _deps(self, eng, reads, writes, same_eng_sync):
        for b in reads:
            if b.w is not None and (same_eng_sync or b.w[2] != eng):
                self._wait(eng, b.w)
            if b.excl:
                for tk in b.r:
                    if tk[2] != eng:
                        self._wait(eng, tk)
        for b in writes:
            if b.w is not None and (same_eng_sync or b.w[2] != eng):
                self._wait(eng, b.w)
            for tk in b.r:
                if tk[2] != eng:
                    self._wait(eng, tk)

    def op(self, eng, fn, reads=(), writes=(), sync_same=None):
        if sync_same is None:
            sync_same = eng != "pe"
        self._deps(eng, reads, writes, sync_same)
        ins = fn(self.E[eng])
        self.cnt[eng] += 1
        ins.then_inc(self.sem[eng], 1)
        self.ninst += 1
        tk = (self.sem[eng], self.cnt[eng], eng)
        for b in reads:
            b.r.append(tk)
            if len(b.r) > 12:
                b.r = b.r[-12:] if False else self._compact(b.r)
        for b in writes:
            b.w = tk
            b.r = []
        return tk

    @staticmethod
    def _compact(r):
        d = {}
        for tk in r:
            k = id(tk[0])
            if k not in d or d[k][1] < tk[1]:
                d[k] = tk
        return list(d.values())

    def dma(self, q, out_ap, in_ap, reads=(), writes=(), semb=None, **kw):
        self._deps(q, reads, writes, True)
        tgt = semb if semb is not None else (writes[0] if writes else reads[0])
        ins = self.E[q].dma_start(out=out_ap, in_=in_ap, **kw)
        if q == "pool":
            if tgt.dsem2 is None:
                tgt.dsem2 = self.newsem("w_" + tgt.name)
            tgt.dcnt2 += 16
            ins.then_inc(tgt.dsem2, 16)
            tk = (tgt.dsem2, tgt.dcnt2, "dma")
        else:
            if tgt.dsem is None:
                tgt.dsem = self.newsem("d_" + tgt.name)
            tgt.dcnt += 16
            ins.then_inc(tgt.dsem, 16)
            tk = (tgt.dsem, tgt.dcnt, "dma")
        self.ninst += 1
        self.dsems[id(tk[0])] = tk
        for b in reads:
            b.r.append(tk)
            if len(b.r) > 12:
                b.r = self._compact(b.r)
        for b in writes:
            b.w = tk
            b.r = []
        return tk

    def finish(self, bufs, q="sp"):
        for b in bufs:
            self._wait(q, b.w)

    def barrier(self):
        for e in self.ENG:
            for p in self.ENG:
                if p != e and self.cnt[p] > 0:
                    self._wait(e, (self.sem[p], self.cnt[p], p))
            for tk in self.dsems.values():
                self._wait(e, tk)

    def idma(self, out_ap, out_off, in_ap, in_off, bound, reads=(), writes=(), semb=None):
        q = "pool"
        self._deps(q, reads, writes, True)
        tgt = semb
        if tgt.dsem2 is None:
            tgt.dsem2 = self.newsem("w_" + tgt.name)
        oo = bass.IndirectOffsetOnAxis(ap=out_off, axis=0) if out_off is not None else None
        io = bass.IndirectOffsetOnAxis(ap=in_off, axis=0) if in_off is not None else None
        if not hasattr(self, "_bregs"):
            self._bregs = {}
        if bound not in self._bregs:
            self._bregs[bound] = self.nc.gpsimd.to_reg(bound)
        ins = self.nc.gpsimd.indirect_dma_start(out=out_ap, out_offset=oo, in_=in_ap, in_offset=io,
                                                bounds_check=self._bregs[bound], oob_is_err=False)
        tgt.dcnt2 += 16
        ins.then_inc(tgt.dsem2, 16)
        self.ninst += 1
        tk = (tgt.dsem2, tgt.dcnt2, "dma")
        self.dsems[id(tgt.dsem2)] = tk
        if not hasattr(self, "idq"):
            self.idq = []
        self.idq.append(tk)
        while len(self.idq) > 16:
            self._wait("pool", self.idq.pop(0))
        for b in reads:
            b.r.append(tk)
            if len(b.r) > 12:
                b.r = self._compact(b.r)
        for b in writes:
            b.w = tk
            b.r = []
        return tk


import os
STOP = int(os.environ.get('GDN_STOP', '0'))
EPS = 1e-6
_epsb = {}


def rsqrt(sc, ob, o_ap, ib, i_ap, scale, eps=EPS):
    eb = _epsb["b"]
    sc.op("act", lambda e: e.activation(out=o_ap, in_=i_ap, func=AF.Sqrt, bias=eb[:o_ap.shape[0], 0:1], scale=scale), reads=[ib, eb], writes=[ob])
    sc.op("dve", lambda e: e.reciprocal(out=o_ap, in_=o_ap), reads=[ob], writes=[ob])


def init_consts(sc):
    eb = sc.sb("epsb", [128, 1], F32)
    sc.op("dve", lambda e: e.memset(eb[:, :], EPS), writes=[eb])
    _epsb["b"] = eb


def proj_phase(nc, sc, S, x, w, gmix_d, fT, tm, gates, ident_bf):
    NB = S // 512
    with ExitStack() as st:
        sc.stack, old = st, sc.stack
        W = sc.sb("W", [128, 16, 1800], BF16)
        gm = sc.sb("gm", [128, 16], F32)
        wst = [sc.sb(f"wst{i}", [128, 1800], F32) for i in range(2)]
        sc.dma("sp", gm[:, :], gmix_d[:, :], writes=[gm])
        for c in range(16):
            b = wst[c % 2]
            sc.dma("sp" if c % 2 == 0 else "act", b[:, :], w[c * 128:(c + 1) * 128, :], writes=[b])
            sc.op("dve", lambda e: e.tensor_scalar(out=W[:, c, :], in0=b[:, :], scalar1=gm[:, c:c + 1], scalar2=None,
                                                   op0=ALU.mult), reads=[b, gm], writes=[W])
        xt = [sc.sb(f"xt{i}", [128, 2048], F32) for i in range(2)]
        junk = sc.sb("junk", [128, 2048], BF16)
        xn = [sc.sb(f"xn{i}", [128, 2048], BF16) for i in range(2)]
        ss = [sc.sb(f"ss{i}", [128, 1], F32) for i in range(2)]
        rs = [sc.sb(f"rs{i}", [128, 1], F32) for i in range(2)]
        hT = [sc.sb(f"hT{i}", [128, 16, 512], BF16) for i in range(2)]
        pT = [sc.ps(f"pT{i}", [128, 1024], BF16) for i in range(2)]
        pf = [sc.ps(f"pf{i}", [128, 512], F32) for i in range(3)]
        pg = sc.ps("pg", [128, 8], F32)
        fst = [sc.sb(f"fst{i}", [128, 10, 512], BF16) for i in range(2)]
        tst = [sc.sb(f"tst{i}", [128, 4, 512], BF16) for i in range(2)]
        gst = [sc.sb(f"gst{i}", [128, 4, 8], F32) for i in range(2)]
        ti = 0
        ev = 0
        for blk in range(NB):
            hb = hT[blk % 2]
            fs = fst[blk % 2]
            ts_ = tst[blk % 2]
            gs = gst[blk % 2]
            for j in range(4):
                t = blk * 4 + j
                xb = xt[t % 2]
                sc.dma("sp" if t % 2 == 0 else "act", xb[:, :], x[t * 128:(t + 1) * 128, :], writes=[xb])
                s_ = ss[t % 2]
                r_ = rs[t % 2]
                sc.op("act", lambda e: e.activation(out=junk[:, :], in_=xb[:, :], func=AF.Square, accum_out=s_[:, :]),
                      reads=[xb], writes=[junk, s_])
                rsqrt(sc, r_, r_[:, :], s_, s_[:, :], 1.0 / 2048)
                xnb = xn[t % 2]
                sc.op("act", lambda e: e.activation(out=xnb[:, :], in_=xb[:, :], func=AF.Copy, scale=r_[:, 0:1]),
                      reads=[xb, r_], writes=[xnb])
                for half in range(2):
                    p = pT[half]
                    for c8 in range(8):
                        c = half * 8 + c8
                        sc.op("pe", lambda e: e.transpose(out=p[:, c8 * 128:(c8 + 1) * 128],
                                                          in_=xnb[:, c * 128:(c + 1) * 128], identity=ident_bf[:, :]),
                              reads=[xnb, ident_bf], writes=[p])
                    eng = "dve" if half == 0 else "act"
                    dst = hb[:, half * 8:(half + 1) * 8, j * 128:(j + 1) * 128]
                    src = p[:, :].rearrange("p (c t) -> p c t", c=8)
                    if eng == "dve":
                        sc.op("dve", lambda e: e.tensor_copy(out=dst, in_=src), reads=[p], writes=[hb])
                    else:
                        sc.op("act", lambda e: e.activation(out=dst, in_=src, func=AF.Copy), reads=[p], writes=[hb])
            for g in range(10):
                p = pf[ev % 3]
                for c in range(16):
                    sc.op("pe", lambda e: e.matmul(p[:, :], lhsT=W[:, c, g * 128:(g + 1) * 128], rhs=hb[:, c, :],
                                                   start=(c == 0), stop=(c == 15)), reads=[W, hb], writes=[p])
                if ev % 2 == 0:
                    sc.op("dve", lambda e: e.tensor_copy(out=fs[:, g, :], in_=p[:, :]), reads=[p], writes=[fs])
                else:
                    sc.op("act", lambda e: e.activation(out=fs[:, g, :], in_=p[:, :], func=AF.Copy), reads=[p], writes=[fs])
                ev += 1
            sc.dma("pool", fT[:, :, blk * 512:(blk + 1) * 512].rearrange("g p t -> p g t"), fs[:, :, :], reads=[fs], writes=[fT_buf], semb=fs)
            for j in range(4):
                p = pf[ev % 3]
                for c in range(16):
                    sc.op("pe", lambda e: e.matmul(p[:, :], lhsT=hb[:, c, j * 128:(j + 1) * 128], rhs=W[:, c, 1280:1792],
                                                   start=(c == 0), stop=(c == 15)), reads=[W, hb], writes=[p])
                for c in range(16):
                    sc.op("pe", lambda e: e.matmul(pg[:, :], lhsT=hb[:, c, j * 128:(j + 1) * 128], rhs=W[:, c, 1792:1800],
                                                   start=(c == 0), stop=(c == 15)), reads=[W, hb], writes=[pg])
                if ev % 2 == 0:
                    sc.op("dve", lambda e: e.tensor_copy(out=ts_[:, j, :], in_=p[:, :]), reads=[p], writes=[ts_])
                else:
                    sc.op("act", lambda e: e.activation(out=ts_[:, j, :], in_=p[:, :], func=AF.Copy), reads=[p], writes=[ts_])
                ev += 1
                sc.op("dve", lambda e: e.tensor_copy(out=gs[:, j, :], in_=pg[:, :]), reads=[pg], writes=[gs])
            sc.dma("pool", tm[blk * 512:(blk + 1) * 512, :].rearrange("(j p) d -> p j d", p=128), ts_[:, :, :],
                   reads=[ts_], writes=[tm_buf], semb=ts_)
            sc.dma("pool", gates[blk * 512:(blk + 1) * 512, :].rearrange("(j p) d -> p j d", p=128), gs[:, :, :],
                   reads=[gs], writes=[gates_buf], semb=gs)
        sc.barrier()
        sc.stack = old


def set_dram_bufs(fT_b, tm_b, gates_b):
    global fT_buf, tm_buf, gates_buf
    fT_buf, tm_buf, gates_buf = fT_b, tm_b, gates_b


def da_phase(nc, sc, S, fT, tm, mix, bias6_d, cst_d, lamv_d, sub_d, ident_bf, ones32, hlist=(0, 1)):
    NQ = S // 512
    NK = S // 128
    with ExitStack() as st:
        sc.stack, old = st, sc.stack
        KT = sc.sb("KT", [128, S], BF16)
        QT = sc.sb("QT", [128, S], BF16)
        V = sc.sb("V", [128, NK, 128], BF16)
        b6 = sc.sb("b6", [128, 6, 512], F32)
        cst = sc.sb("cst", [128, 2], F32)
        lamv = sc.sb("lamv", [128, 4, 64], F32)
        lt = sc.sb("lt", [128, 4], F32)
        nlam = sc.sb("nlam", [128, 1], F32)
        sub = sc.sb("sub", [128, 1], F32)
        ljunk = sc.sb("ljunk", [128, 64], F32)
        sc.dma("sp", lamv[:, :, :], lamv_d[:, :, :], writes=[lamv])
        sc.dma("sp", sub[:, :], sub_d[:, :], writes=[sub])
        for i in range(2):
            sc.op("dve", lambda e: e.tensor_tensor(out=ljunk[:, :], in0=lamv[:, 2 * i, :], in1=lamv[:, 2 * i + 1, :], op=ALU.mult),
                  reads=[lamv], writes=[ljunk])
            sc.op("dve", lambda e: e.reduce_sum(out=lt[:, i:i + 1], in_=ljunk[:, :], axis=AX.X), reads=[ljunk], writes=[lt])
        sc.op("act", lambda e: e.activation(out=lt[:, 2:4], in_=lt[:, 0:2], func=AF.Exp), reads=[lt], writes=[lt])
        sc.op("dve", lambda e: e.scalar_tensor_tensor(out=nlam[:, :], in0=lt[:, 3:4], scalar=-0.2, in1=lt[:, 2:3],
                                                      op0=ALU.add, op1=ALU.subtract), reads=[lt], writes=[nlam])
        pS = [sc.ps(f"pS{i}", [128, 512], F32) for i in range(4)]
        pO = [sc.ps(f"pO{i}", [128, 512], F32) for i in range(2)]
        pX = [sc.ps(f"pX{i}", [128, 512], F32) for i in range(2)]
        PT = [sc.sb(f"PT{i}", [128, 512], BF16) for i in range(4)]
        tmpb = [sc.sb(f"tmpb{i}", [128, 512], F32) for i in range(2)]
        acc = [sc.sb(f"acc{i}", [128, 512], F32) for i in range(2)]
        rl = [sc.sb(f"rl{i}", [128, 512], F32) for i in range(2)]
        o1 = sc.sb("o1", [128, 512], F32)
        o2 = sc.sb("o2", [128, 512], F32)
        sq = sc.sb("sq", [128, 512], F32)
        onb = sc.sb("onb", [128, 512], BF16)
        ost = [sc.sb(f"ost{i}", [128, 4, 128], BF16) for i in range(2)]
        it = 0
        for h in hlist:
            sc.dma("sp", KT[:, :], fT[2 + h, :, :], writes=[KT])
            sc.dma("act", QT[:, :], fT[h, :, :], writes=[QT])
            for vq in range(4):
                n0, n1 = vq * NK // 4, (vq + 1) * NK // 4
                if n1 > n0:
                    sc.dma("pool", V[:, n0:n1, :], tm[n0 * 128:n1 * 128, h * 128:(h + 1) * 128].rearrange("(n p) d -> p n d", p=128), writes=[V])
            sc.dma("sp", b6[:, :, :], bias6_d[h, :, :, :], writes=[b6])
            sc.dma("sp", cst[:, :], cst_d[h, :, :], writes=[cst])
            for Q in range(NQ):
                for kb in range(NK):
                    rel = kb - 4 * Q
                    for m in range(2):
                        ps = pS[it % 4]
                        pt = PT[it % 4]
                        it += 1
                        sc.op("pe", lambda e: e.matmul(ps[:, :], lhsT=KT[64 * m:64 * m + 64, kb * 128:(kb + 1) * 128],
                                                       rhs=QT[64 * m:64 * m + 64, Q * 512:(Q + 1) * 512], start=True, stop=True),
                              reads=[KT, QT], writes=[ps])
                        if rel < -1 or rel > 4:
                            ci = 0 if rel < -1 else 1
                            sc.op("act", lambda e: e.activation(out=pt[:, :], in_=ps[:, :], func=AF.Exp,
                                                                bias=cst[:, ci:ci + 1], scale=0.125),
                                  reads=[ps, cst], writes=[pt])
                        else:
                            tb = tmpb[m]
                            sc.op("dve", lambda e: e.scalar_tensor_tensor(out=tb[:, :], in0=ps[:, :], scalar=0.125,
                                                                          in1=b6[:, rel + 1, :], op0=ALU.mult, op1=ALU.add),
                                  reads=[ps, b6], writes=[tb])
                            sc.op("act", lambda e: e.activation(out=pt[:, :], in_=tb[:, :], func=AF.Exp),
                                  reads=[tb], writes=[pt])
                        sc.op("pe", lambda e: e.matmul(pO[m][:, :], lhsT=V[:, kb, :], rhs=pt[:, :], start=(kb == 0),
                                                       stop=(kb == NK - 1)), reads=[V, pt], writes=[pO[m]])
                        if kb == 0:
                            sc.op("dve", lambda e: e.tensor_copy(out=acc[m][:, :], in_=pt[:, :]), reads=[pt], writes=[acc[m]])
                        else:
                            sc.op("dve", lambda e: e.tensor_tensor(out=acc[m][:, :], in0=acc[m][:, :], in1=pt[:, :], op=ALU.add),
                                  reads=[pt, acc[m]], writes=[acc[m]], sync_same=False)
                for m in range(2):
                    sc.op("pe", lambda e: e.matmul(pX[m][:, :], lhsT=ones32[:, :], rhs=acc[m][:, :], start=True, stop=True),
                          reads=[ones32, acc[m]], writes=[pX[m]])
                    sc.op("dve", lambda e: e.reciprocal(out=rl[m][:, :], in_=pX[m][:, :]), reads=[pX[m]], writes=[rl[m]])
                sc.op("dve", lambda e: e.tensor_tensor(out=o1[:, :], in0=pO[0][:, :], in1=rl[0][:, :], op=ALU.mult),
                      reads=[pO[0], rl[0]], writes=[o1])
                sc.op("dve", lambda e: e.tensor_tensor(out=o2[:, :], in0=pO[1][:, :], in1=rl[1][:, :], op=ALU.mult),
                      reads=[pO[1], rl[1]], writes=[o2])
                sc.op("dve", lambda e: e.scalar_tensor_tensor(out=o1[:, :], in0=o2[:, :], scalar=nlam[:, 0:1], in1=o1[:, :],
                                                              op0=ALU.mult, op1=ALU.add), reads=[o2, o1, nlam], writes=[o1])
                sc.op("act", lambda e: e.activation(out=sq[:, :], in_=o1[:, :], func=AF.Square), reads=[o1], writes=[sq])
                sc.op("pe", lambda e: e.matmul(pX[0][:, :], lhsT=ones32[:, :], rhs=sq[:, :], start=True, stop=True),
                      reads=[ones32, sq], writes=[pX[0]])
                rsqrt(sc, sq, sq[:, :], pX[0], pX[0][:, :], 1.0 / 128)
                sc.op("dve", lambda e: e.tensor_scalar(out=sq[:, :], in0=sq[:, :], scalar1=0.8, scalar2=None, op0=ALU.mult),
                      reads=[sq], writes=[sq])
                sc.op("dve", lambda e: e.scalar_tensor_tensor(out=onb[:, :], in0=o1[:, :], scalar=sub[:, 0:1], in1=sq[:, :],
                                                              op0=ALU.mult, op1=ALU.mult), reads=[o1, sub, sq], writes=[onb])
                pTr = pX[1]
                os_ = ost[Q % 2]
                ptb = pTr[:, 0:256].bitcast(BF16)
                for j in range(4):
                    sc.op("pe", lambda e: e.transpose(out=ptb[:, j * 128:(j + 1) * 128], in_=onb[:, j * 128:(j + 1) * 128],
                                                      identity=ident_bf[:, :]), reads=[onb, ident_bf], writes=[pTr])
                sc.op("act", lambda e: e.activation(out=os_[:, :, :], in_=ptb.rearrange("p (j d) -> p j d", j=4), func=AF.Copy),
                      reads=[pTr], writes=[os_])
                sc.dma("sp", mix[Q * 512:(Q + 1) * 512, h * 128:(h + 1) * 128].rearrange("(j p) d -> p j d", p=128), os_[:, :, :],
                       reads=[os_], writes=[mix_buf], semb=os_)
            sc.barrier()
        sc.stack = old


def set_mix_buf(b):
    global mix_buf
    mix_buf = b


def gdn_phase(nc, sc, S, fT, tm, gates, mix, cw_d, gpar_d, gnorm_d, cm_d, ident_bf, id32, ones32, hlist=(0, 1), dbg=None):
    NC = S // 128
    CB = min(2048, S)
    QS = 128 ** -0.5
    with ExitStack() as st:
        sc.stack, old = st, sc.stack
        cm = sc.sb("cm", [128, 10, 128], F32)
        sc.dma("sp", cm[:, :, :], cm_d.rearrange("c p f -> p c f"), writes=[cm])
        onesb = sc.sb("onesb", [128, 128], BF16)
        sc.op("dve", lambda e: e.memset(onesb[:, :], 1.0), writes=[onesb])
        gnorm = sc.sb("gnorm", [128, 128], F32)
        sc.dma("sp", gnorm[:, :], gnorm_d[:, :], writes=[gnorm])
        qT = sc.sb("qT", [128, S], BF16)
        kT = sc.sb("kT", [128, S], BF16)
        vtok = sc.sb("vtok", [128, NC, 128], BF16)
        for hg in hlist:
            with ExitStack() as st2:
                sc.stack = st2
                cw = sc.sb("cw", [128, 3, 5], F32)
                for a in range(3):
                    sc.dma("sp", cw[:, a, :], cw_d[2 * a + hg, :, :], writes=[cw])
                ub = [sc.sb(f"ub{i}", [128, CB + 4], BF16) for i in range(2)]
                ca = sc.sb("ca", [128, CB], F32)
                cc = sc.sb("cc", [128, CB], F32)
                csq = sc.sb("csq", [128, CB], BF16)
                rin = sc.sb("rin", [128, 512], F32)
                vT = sc.sb("vT", [128, CB], BF16)
                pc = [sc.ps(f"pc{i}", [128, 512], F32) for i in range(2)]
                pv = sc.ps("pv", [128, 1024], BF16)
                ui = 0
                for a, dst in ((0, qT), (1, kT), (2, None)):
                    src = fT[4 + 2 * a + hg, :, :]
                    for cb in range(S // CB):
                        u = ub[ui % 2]
                        ui += 1
                        lo = cb * CB - 2
                        hi = cb * CB + CB + 2
                        l2, h2 = max(lo, 0), min(hi, S)
                        if lo < 0:
                            sc.op("pool", lambda e: e.memset(u[:, 0:2], 0.0), writes=[u])
                        if hi > S:
                            sc.op("pool", lambda e: e.memset(u[:, CB + 2:CB + 4], 0.0), writes=[u])
                        sc.dma("sp", u[:, l2 - lo:h2 - lo], src[:, l2:h2], writes=[u])
                        sc.op("dve", lambda e: e.tensor_scalar(out=ca[:, :], in0=u[:, 0:CB], scalar1=cw[:, a, 0:1], scalar2=None,
                                                               op0=ALU.mult), reads=[u, cw], writes=[ca])
                        for j in range(1, 5):
                            sc.op("dve", lambda e: e.scalar_tensor_tensor(out=ca[:, :], in0=u[:, j:j + CB], scalar=cw[:, a, j:j + 1],
                                                                          in1=ca[:, :], op0=ALU.mult, op1=ALU.add),
                                  reads=[u, cw, ca], writes=[ca])
                        if dst is None:
                            sc.op("act", lambda e: e.activation(out=vT[:, :], in_=ca[:, :], func=AF.Silu), reads=[ca], writes=[vT])
                            for t8 in range(CB // 1024):
                                for t in range(8):
                                    tt = t8 * 8 + t
                                    sc.op("pe", lambda e: e.transpose(out=pv[:, t * 128:(t + 1) * 128], in_=vT[:, tt * 128:(tt + 1) * 128],
                                                                      identity=ident_bf[:, :]), reads=[vT, ident_bf], writes=[pv])
                                c0 = cb * (CB // 128) + t8 * 8
                                sc.op("act", lambda e: e.activation(out=vtok[:, c0:c0 + 8, :], in_=pv[:, :].rearrange("p (c d) -> p c d", c=8),
                                                                    func=AF.Copy), reads=[pv], writes=[vtok])
                            continue
                        sc.op("act", lambda e: e.activation(out=cc[:, :], in_=ca[:, :], func=AF.Silu), reads=[ca], writes=[cc])
                        sc.op("pool", lambda e: e.tensor_tensor(out=csq[:, :], in0=cc[:, :], in1=cc[:, :], op=ALU.mult), reads=[cc], writes=[csq])
                        for s4 in range(CB // 512):
                            p = pc[s4 % 2]
                            sl = slice(s4 * 512, (s4 + 1) * 512)
                            sc.op("pe", lambda e: e.matmul(p[:, :], lhsT=onesb[:, :], rhs=csq[:, sl], start=True, stop=True),
                                  reads=[onesb, csq], writes=[p])
                            rsqrt(sc, rin, rin[:, :], p, p[:, :], 1.0)
                            sc.op("dve", lambda e: e.tensor_tensor(out=dst[:, cb * CB + s4 * 512:cb * CB + (s4 + 1) * 512], in0=cc[:, sl],
                                                                   in1=rin[:, :], op=ALU.mult), reads=[cc, rin], writes=[dst])
                sc.barrier()
            if STOP == 1:
                continue
            with ExitStack() as st3:
                sc.stack = st3
                gt = sc.sb("gt", [128, NC, 8], F32)
                for vq in range(4):
                    n0, n1 = vq * NC // 4, (vq + 1) * NC // 4
                    if n1 > n0:
                        sc.dma("sp", gt[:, n0:n1, :], gates[n0 * 128:n1 * 128, :].rearrange("(n p) g -> p n g", p=128), writes=[gt])
                gp = sc.sb("gp", [128, 4], F32)
                sc.dma("sp", gp[:, :], gpar_d[hg, :, :], writes=[gp])
                nea = sc.sb("nea", [128, 2], F32)
                sc.op("act", lambda e: e.activation(out=nea[:, 0:1], in_=gp[:, 0:1], func=AF.Exp), reads=[gp], writes=[nea])
                sc.op("act", lambda e: e.activation(out=nea[:, 1:2], in_=gp[:, 2:3], func=AF.Exp), reads=[gp], writes=[nea])
                sc.op("dve", lambda e: e.tensor_scalar(out=nea[:, :], in0=nea[:, :], scalar1=-1.0, scalar2=None, op0=ALU.mult),
                      reads=[nea], writes=[nea])
                G = {}
                pg1, pg2 = sc.ps_multi("pg", 2, width=128)
                pkk = sc.ps_multi("pk", 2, width=128, dt=BF16)
                pg1 = Buf(View(pg1.t.t, pg1.t.off, NC), "pg1", root=pg1.root, excl=True)
                pg2 = Buf(View(pg2.t.t, pg2.t.off, NC), "pg2", root=pg2.root, excl=True)
                for d in range(2):
                    ga = sc.sb(f"ga{d}", [128, NC], F32)
                    beta = sc.sb(f"beta{d}", [128, NC], F32)
                    nbeta = sc.sb(f"nbeta{d}", [128, NC], F32)
                    gc = sc.sb(f"gc{d}", [128, NC], F32)
                    ngc = sc.sb(f"ngc{d}", [128, NC], F32)
                    gam = sc.sb(f"gam{d}", [128, NC], F32)
                    ngam = sc.sb(f"ngam{d}", [128, NC], F32)
                    gams = sc.sb(f"gams{d}", [128, NC], F32)
                    gC = sc.sb(f"gC{d}", [128, NC], F32)
                    kdec = sc.sb(f"kdec{d}", [128, NC], F32)
                    acol = (0 if d == 0 else 4) + hg
                    bcol = (2 if d == 0 else 6) + hg
                    sc.op("act", lambda e: e.activation(out=ga[:, :], in_=gt[:, :, acol], func=AF.Exp, bias=gp[:, 2 * d + 1:2 * d + 2], scale=1.0),
                          reads=[gt, gp], writes=[ga])
                    sc.op("act", lambda e: e.activation(out=ga[:, :], in_=ga[:, :], func=AF.Ln, bias=1.0, scale=1.0), reads=[ga], writes=[ga])
                    sc.op("dve", lambda e: e.tensor_scalar(out=ga[:, :], in0=ga[:, :], scalar1=nea[:, d:d + 1], scalar2=None, op0=ALU.mult),
                          reads=[ga, nea], writes=[ga])
                    sc.op("act", lambda e: e.activation(out=beta[:, :], in_=gt[:, :, bcol], func=AF.Sigmoid), reads=[gt], writes=[beta])
                    sc.op("dve", lambda e: e.tensor_scalar(out=nbeta[:, :], in0=beta[:, :], scalar1=-1.0, scalar2=None, op0=ALU.mult),
                          reads=[beta], writes=[nbeta])
                    sc.op("pe", lambda e: e.matmul(pg1[:, :], lhsT=cm[:, d, :], rhs=ga[:, :], start=True, stop=True), reads=[cm, ga], writes=[pg1])
                    sc.op("dve", lambda e: e.tensor_copy(out=gc[:, :], in_=pg1[:, :]), reads=[pg1], writes=[gc])
                    sc.op("dve", lambda e: e.tensor_scalar(out=ngc[:, :], in0=pg1[:, :], scalar1=-1.0, scalar2=None, op0=ALU.mult),
                          reads=[pg1], writes=[ngc])
                    sc.op("act", lambda e: e.activation(out=gam[:, :], in_=pg1[:, :], func=AF.Exp), reads=[pg1], writes=[gam])
                    sc.op("dve", lambda e: e.tensor_scalar(out=ngam[:, :], in0=gam[:, :], scalar1=-1.0, scalar2=None, op0=ALU.mult),
                          reads=[gam], writes=[ngam])
                    sc.op("dve", lambda e: e.tensor_scalar(out=gams[:, :], in0=gam[:, :], scalar1=QS, scalar2=None, op0=ALU.mult),
                          reads=[gam], writes=[gams])
                    sc.op("pe", lambda e: e.matmul(pg2[:, :], lhsT=cm[:, 2 + d, :], rhs=gc[:, :], start=True, stop=True), reads=[cm, gc], writes=[pg2])
                    sc.op("act", lambda e: e.activation(out=gC[:, :], in_=pg2[:, :], func=AF.Exp), reads=[pg2], writes=[gC])
                    sc.op("dve", lambda e: e.tensor_tensor(out=kdec[:, :], in0=pg2[:, :], in1=gc[:, :], op=ALU.subtract), reads=[pg2, gc], writes=[kdec])
                    sc.op("act", lambda e: e.activation(out=kdec[:, :], in_=kdec[:, :], func=AF.Exp), reads=[kdec], writes=[kdec])
                    G[d] = dict(beta=beta, nbeta=nbeta, gc=gc, ngc=ngc, gam=gam, ngam=ngam, gams=gams, gC=gC, kdec=kdec)
                if STOP == 2:
                    sc.barrier()
                    continue
                oacc = sc.sb("oacc", [128, NC, 128], F32)
                D = {}
                for d in range(2):
                    pb1 = sc.ps_multi(f"pb1{d}", 4)
                    pb2 = sc.ps_multi(f"pb2{d}", 4)
                    pb3 = sc.ps_multi(f"pb3{d}", 3)
                    D[d] = dict(
                        AT=[sc.sb(f"AT{d}{i}", [128, 128], BF16) for i in range(2)],
                        TT=[sc.sb(f"TT{d}{i}", [128, 128], BF16) for i in range(2)],
                        kt=[sc.sb(f"kt{d}{i}", [128, 128], BF16) for i in range(2)],
                        S32=sc.sb(f"S32{d}", [128, 128], F32), Sb=sc.sb(f"Sb{d}", [128, 128], BF16),
                        Dg=sc.sb(f"Dg{d}", [128, 128], F32), decT=sc.sb(f"decT{d}", [128, 128], F32),
                        Y=[sc.sb(f"Y{d}{i}", [128, 128], F32) for i in range(2)],
                        YT=[sc.sb(f"YT{d}{i}", [128, 128], F32) for i in range(2)],
                        Q=[sc.sb(f"Q{d}{i}", [128, 128], F32) for i in range(2)],
                        rp=sc.sb(f"rp{d}", [128, 128], BF16), vn=sc.sb(f"vn{d}", [128, 128], BF16),
                        P2s=sc.sb(f"P2s{d}", [128, 128], F32), otmp=sc.sb(f"otmp{d}", [128, 128], F32),
                        pA=pb1[0], pB=pb1[1], pC=pb1[2], pN=[pb1[3], pb2[0], pb2[1]], pk=pkk[d],
                        p1=pb2[2], p2=pb2[3], p3=pb3[0], p4=pb3[1], p5=pb3[2], ni=0)
                    sc.op("pool", lambda e: e.memset(D[d]["S32"][:, :], 0.0), writes=[D[d]["S32"]])
                    sc.op("pool", lambda e: e.memset(D[d]["Sb"][:, :], 0.0), writes=[D[d]["Sb"]])

                def pre(d, n):
                    c = n if d == 0 else NC - 1 - n
                    cs = slice(c * 128, (c + 1) * 128)
                    g = G[d]
                    b = D[d]
                    AT, TT, kt = b["AT"][n % 2], b["TT"][n % 2], b["kt"][n % 2]
                    pA, pB, pC = b["pA"], b["pB"], b["pC"]
                    sc.op("pe", lambda e: e.matmul(pA[:, :], lhsT=kT[:, cs], rhs=kT[:, cs], start=True, stop=True), reads=[kT], writes=[pA])
                    sc.op("pe", lambda e: e.matmul(pC[:, :], lhsT=kT[:, cs], rhs=qT[:, cs], start=True, stop=True), reads=[kT, qT], writes=[pC])
                    if STOP == 31: return
                    sc.op("pool", lambda e: e.tensor_scalar(out=b["Dg"][:, :], in0=id32[:, :], scalar1=g["gc"][:, c:c + 1], scalar2=None, op0=ALU.mult),
                          reads=[id32, g["gc"]], writes=[b["Dg"]])
                    if STOP == 32: return
                    sc.op("pe", lambda e: e.matmul(pB[:, :], lhsT=ones32[:, :], rhs=b["Dg"][:, :], start=True, stop=False), reads=[ones32, b["Dg"]], writes=[pB])
                    sc.op("pe", lambda e: e.matmul(pB[:, :], lhsT=id32[:, :], rhs=cm[:, 4 + d, :], start=False, stop=True), reads=[id32, cm], writes=[pB])
                    decT = b["decT"]
                    sc.op("act", lambda e: e.activation(out=decT[:, :], in_=pB[:, :], func=AF.Exp, bias=g["ngc"][:, c:c + 1], scale=1.0),
                          reads=[pB, g["ngc"]], writes=[decT])
                    if STOP == 33: return
                    sc.op("dve", lambda e: e.scalar_tensor_tensor(out=AT[:, :], in0=pC[:, :], scalar=QS, in1=decT[:, :], op0=ALU.mult, op1=ALU.mult),
                          reads=[pC, decT], writes=[AT])
                    Y0, Y1 = b["Y"]
                    YT0, YT1 = b["YT"]
                    Q0, Q1 = b["Q"]
                    sc.op("dve", lambda e: e.scalar_tensor_tensor(out=Y0[:, :], in0=pA[:, :], scalar=g["nbeta"][:, c:c + 1], in1=decT[:, :],
                                                                  op0=ALU.mult, op1=ALU.mult), reads=[pA, g["nbeta"], decT], writes=[Y0])
                    sc.op("pool", lambda e: e.tensor_tensor(out=Y0[:, :], in0=Y0[:, :], in1=cm[:, 6 + d, :], op=ALU.mult), reads=[Y0, cm], writes=[Y0])
                    if STOP == 34: return
                    pn = b["pN"]
                    ni = b["ni"]
                    p = pn[ni % 3]; ni += 1
                    sc.op("pe", lambda e: e.transpose(out=p[:, :], in_=Y0[:, :], identity=id32[:, :]), reads=[Y0, id32], writes=[p])
                    sc.op("act", lambda e: e.activation(out=YT0[:, :], in_=p[:, :], func=AF.Copy), reads=[p], writes=[YT0])
                    sc.op("pool", lambda e: e.tensor_tensor(out=Q0[:, :], in0=Y0[:, :], in1=id32[:, :], op=ALU.add), reads=[Y0, id32], writes=[Q0])
                    if STOP == 35: return
                    Y, YT, Q, Yn, YTn, Qn = Y0, YT0, Q0, Y1, YT1, Q1
                    for k in range(1, 7):
                        p = pn[ni % 3]; ni += 1
                        sc.op("pe", lambda e: e.matmul(p[:, :], lhsT=Y[:, :], rhs=YT[:, :], start=True, stop=True), reads=[Y, YT], writes=[p])
                        if k < 6:
                            p2 = pn[ni % 3]; ni += 1
                            sc.op("pe", lambda e: e.matmul(p2[:, :], lhsT=YT[:, :], rhs=Y[:, :], start=True, stop=True), reads=[Y, YT], writes=[p2])
                        sc.op("act", lambda e: e.activation(out=YTn[:, :], in_=p[:, :], func=AF.Copy), reads=[p], writes=[YTn])
                        if k < 6:
                            sc.op("dve", lambda e: e.tensor_copy(out=Yn[:, :], in_=p2[:, :]), reads=[p2], writes=[Yn])
                        p3 = pn[ni % 3]; ni += 1
                        sc.op("pe", lambda e: e.matmul(p3[:, :], lhsT=YTn[:, :], rhs=Q[:, :], start=True, stop=True), reads=[YTn, Q], writes=[p3])
                        if k < 6:
                            sc.op("dve", lambda e: e.tensor_tensor(out=Qn[:, :], in0=p3[:, :], in1=Q[:, :], op=ALU.add), reads=[p3, Q], writes=[Qn])
                        else:
                            sc.op("dve", lambda e: e.tensor_tensor(out=TT[:, :], in0=p3[:, :], in1=Q[:, :], op=ALU.add), reads=[p3, Q], writes=[TT])
                        Y, YT, Q, Yn, YTn, Qn = Yn, YTn, Qn, Y, YT, Q
                    b["ni"] = ni
                    if STOP == 36: return
                    pk = b["pk"]
                    sc.op("pe", lambda e: e.transpose(out=pk[:, :], in_=kT[:, cs], identity=ident_bf[:, :]), reads=[kT, ident_bf], writes=[pk])
                    sc.op("act", lambda e: e.activation(out=kt[:, :], in_=pk[:, :], func=AF.Copy, scale=g["kdec"][:, c:c + 1]),
                          reads=[pk, g["kdec"]], writes=[kt])

                def step(d, n):
                    c = n if d == 0 else NC - 1 - n
                    cs = slice(c * 128, (c + 1) * 128)
                    g = G[d]
                    b = D[d]
                    AT, TT, kt = b["AT"][n % 2], b["TT"][n % 2], b["kt"][n % 2]
                    S32, Sb, rp, vn, P2s = b["S32"], b["Sb"], b["rp"], b["vn"], b["P2s"]
                    p1, p2, p3, p4, p5 = b["p1"], b["p2"], b["p3"], b["p4"], b["p5"]
                    sc.op("pe", lambda e: e.matmul(p1[:, :], lhsT=kT[:, cs], rhs=Sb[:, :], start=True, stop=True), reads=[kT, Sb], writes=[p1])
                    sc.op("pe", lambda e: e.matmul(p2[:, :], lhsT=qT[:, cs], rhs=Sb[:, :], start=True, stop=True), reads=[qT, Sb], writes=[p2])
                    sc.op("dve", lambda e: e.scalar_tensor_tensor(out=rp[:, :], in0=p1[:, :], scalar=g["ngam"][:, c:c + 1], in1=vtok[:, c, :],
                                                                  op0=ALU.mult, op1=ALU.add), reads=[p1, g["ngam"], vtok], writes=[rp])
                    sc.op("act", lambda e: e.activation(out=P2s[:, :], in_=p2[:, :], func=AF.Copy, scale=g["gams"][:, c:c + 1]),
                          reads=[p2, g["gams"]], writes=[P2s])
                    sc.op("pe", lambda e: e.matmul(p3[:, :], lhsT=TT[:, :], rhs=rp[:, :], start=True, stop=True), reads=[TT, rp], writes=[p3])
                    sc.op("act", lambda e: e.activation(out=vn[:, :], in_=p3[:, :], func=AF.Copy, scale=g["beta"][:, c:c + 1]),
                          reads=[p3, g["beta"]], writes=[vn])
                    sc.op("pe", lambda e: e.matmul(p5[:, :], lhsT=kt[:, :], rhs=vn[:, :], start=True, stop=True), reads=[kt, vn], writes=[p5])
                    sc.op("pe", lambda e: e.matmul(p4[:, :], lhsT=AT[:, :], rhs=vn[:, :], start=True, stop=True), reads=[AT, vn], writes=[p4])
                    sc.op("dve", lambda e: e.scalar_tensor_tensor(out=S32[:, :], in0=S32[:, :], scalar=g["gC"][:, c:c + 1], in1=p5[:, :],
                                                                  op0=ALU.mult, op1=ALU.add), reads=[S32, g["gC"], p5], writes=[S32])
                    sc.op("act", lambda e: e.activation(out=Sb[:, :], in_=S32[:, :], func=AF.Copy), reads=[S32], writes=[Sb])
                    if n < NC // 2:
                        sc.op("pool" if False else "dve", lambda e: e.tensor_tensor(out=oacc[:, c, :], in0=p4[:, :], in1=P2s[:, :], op=ALU.add),
                              reads=[p4, P2s], writes=[oacc])
                    else:
                        ot = b["otmp"]
                        sc.op("dve", lambda e: e.tensor_tensor(out=ot[:, :], in0=p4[:, :], in1=P2s[:, :], op=ALU.add), reads=[p4, P2s], writes=[ot])
                        sc.op("pool", lambda e: e.tensor_tensor(out=oacc[:, c, :], in0=oacc[:, c, :], in1=ot[:, :], op=ALU.add),
                              reads=[ot, oacc], writes=[oacc])

                pre(0, 0)
                if STOP == 3 or STOP > 30:
                    sc.barrier()
                    continue
                pre(1, 0)
                if STOP == 4:
                    sc.barrier()
                    continue
                for n in range(NC):
                    if n + 1 < NC:
                        pre(0, n + 1)
                        pre(1, n + 1)
                    step(0, n)
                    step(1, n)
                if dbg is not None:
                    sc.dma("sp", dbg[hg].rearrange("(n p) d -> p n d", p=128), oacc[:, :, :], reads=[oacc], writes=[Buf(dbg, "dbg")], semb=oacc)
                GS = min(4, NC)
                zt = [sc.sb(f"zt{i}", [128, GS, 128], BF16) for i in range(2)]
                sz = [sc.sb(f"sz{i}", [128, GS, 128], BF16) for i in range(2)]
                og = [sc.sb(f"og{i}", [128, GS, 128], BF16) for i in range(2)]
                ssq = sc.sb("ssq", [128, NC], F32)
                oj = sc.sb("oj", [128, 128], F32)
                for c in range(NC):
                    sc.op("act", lambda e: e.activation(out=oj[:, :], in_=oacc[:, c, :], func=AF.Square, accum_out=ssq[:, c:c + 1]),
                          reads=[oacc], writes=[oj, ssq])
                rsqrt(sc, ssq, ssq[:, :], ssq, ssq[:, :], 1.0 / 128)
                for gi in range(NC // GS):
                    z_, s_, o_ = zt[gi % 2], sz[gi % 2], og[gi % 2]
                    r0 = gi * GS * 128
                    sc.dma("sp", z_[:, :, :], tm[r0:r0 + GS * 128, 256 + hg * 128:256 + (hg + 1) * 128].rearrange("(n p) d -> p n d", p=128), writes=[z_])
                    sc.op("act", lambda e: e.activation(out=s_[:, :, :], in_=z_[:, :, :], func=AF.Silu), reads=[z_], writes=[s_])
                    for cc in range(GS):
                        c = gi * GS + cc
                        sc.op("dve", lambda e: e.scalar_tensor_tensor(out=oj[:, :], in0=oacc[:, c, :], scalar=ssq[:, c:c + 1], in1=gnorm[:, :],
                                                                      op0=ALU.mult, op1=ALU.mult), reads=[oacc, ssq, gnorm], writes=[oj])
                        sc.op("pool", lambda e: e.tensor_tensor(out=o_[:, cc, :], in0=oj[:, :], in1=s_[:, cc, :], op=ALU.mult), reads=[oj, s_], writes=[o_])
                    sc.dma("sp", mix[r0:r0 + GS * 128, 256 + hg * 128:256 + (hg + 1) * 128].rearrange("(n p) d -> p n d", p=128), o_[:, :, :],
                           reads=[o_], writes=[mix_buf], semb=o_)
                sc.barrier()
        sc.stack = old


def norm_T(sc, src_buf, src_ap, xn, ssb, rsb, junk, pT, idb, dstT, dst_j, hm_out=None):
    sc.op("act", lambda e: e.activation(out=junk[:, :], in_=src_ap, func=AF.Square, accum_out=ssb[:, :]), reads=[src_buf], writes=[junk, ssb])
    rsqrt(sc, rsb, rsb[:, :], ssb, ssb[:, :], 1.0 / 2048)
    sc.op("act", lambda e: e.activation(out=xn[:, :], in_=src_ap, func=AF.Copy, scale=rsb[:, 0:1]), reads=[src_buf, rsb], writes=[xn])
    for half in range(2):
        p = pT[half]
        for c8 in range(8):
            c = half * 8 + c8
            sc.op("pe", lambda e: e.transpose(out=p[:, c8 * 128:(c8 + 1) * 128], in_=xn[:, c * 128:(c + 1) * 128], identity=idb[:, :]),
                  reads=[xn, idb], writes=[p])
        dst = dstT[:, half * 8:(half + 1) * 8, dst_j * 128:(dst_j + 1) * 128]
        src = p[:, :].rearrange("p (c t) -> p c t", c=8)
        if half == 0:
            sc.op("dve", lambda e: e.tensor_copy(out=dst, in_=src), reads=[p], writes=[dstT])
        else:
            sc.op("act", lambda e: e.activation(out=dst, in_=src, func=AF.Copy), reads=[p], writes=[dstT])


def tok_phase(nc, sc, T, x, mixT, mem, w_out, w_cq, w_ckv, w_co, w_r, gains, x2o, hmo, affo, idb, onesb):
    NB = T // 512
    o_x2, o_hm, o_aff = Buf(x2o, "x2o"), Buf(hmo, "hmo"), Buf(affo, "affo")
    with ExitStack() as st:
        sc.stack, old = st, sc.stack
        Wo = sc.sb("Wo", [128, 16, 2048], BF16)
        Wq = sc.sb("Wq", [128, 16, 512], BF16)
        Wco = sc.sb("Wco", [128, 4, 2048], BF16)
        Wr = sc.sb("Wr", [128, 16, 16], BF16)
        KT = sc.sb("KTm", [128, 4, 256], BF16)
        Vm = sc.sb("Vm", [128, 2, 512], BF16)
        gn = sc.sb("gn", [128, 3, 16], F32)
        sc.dma("sp", gn[:, :, :], gains.rearrange("g p c -> p g c"), writes=[gn])
        xn = sc.sb("xn", [128, 2048], BF16)
        junk = sc.sb("junk2", [128, 2048], BF16)
        ssb = sc.sb("ssb", [128, 1], F32)
        rsb = sc.sb("rsb", [128, 1], F32)
        pT = [sc.ps(f"pT2{i}", [128, 1024], BF16) for i in range(2)]
        pb = [sc.ps(f"pb{i}", [128, 512], F32) for i in range(3)]
        psm = sc.ps("psm", [128, 512], F32)
        pl = sc.ps("pl", [128, 16], F32)
        with ExitStack() as st2:
            sc.stack = st2
            stg = [sc.sb(f"stg{i}", [128, 2048], F32) for i in range(2)]
            Wkv = sc.sb("Wkv", [128, 16, 1024], BF16)
            hmT = sc.sb("hmT", [128, 16, 256], BF16)
            k = 0
            for c in range(16):
                for (src, ncol, dst, gi) in ((w_out, 2048, Wo, None), (w_cq, 512, Wq, 0), (w_ckv, 1024, Wkv, 1), (w_r, 16, Wr, 2)):
                    b = stg[k % 2]; k += 1
                    sc.dma("sp" if k % 2 else "act", b[:, 0:ncol], src[c * 128:(c + 1) * 128, :], writes=[b])
                    if gi is None:
                        sc.op("dve", lambda e: e.tensor_copy(out=dst[:, c, :], in_=b[:, 0:ncol]), reads=[b], writes=[dst])
                    else:
                        sc.op("dve", lambda e: e.tensor_scalar(out=dst[:, c, :], in0=b[:, 0:ncol], scalar1=gn[:, gi, c:c + 1], scalar2=None,
                                                               op0=ALU.mult), reads=[b, gn], writes=[dst])
            for h in range(4):
                b = stg[k % 2]; k += 1
                sc.dma("sp", b[:, :], w_co[h * 128:(h + 1) * 128, :], writes=[b])
                sc.op("dve", lambda e: e.tensor_copy(out=Wco[:, h, :], in_=b[:, :]), reads=[b], writes=[Wco])
            for t in range(2):
                b = stg[k % 2]; k += 1
                sc.dma("sp", b[:, :], mem[t * 128:(t + 1) * 128, :], writes=[b])
                norm_T(sc, b, b[:, :], xn, ssb, rsb, junk, pT, idb, hmT, t)
            for h in range(4):
                p = pb[h % 3]
                for c in range(16):
                    sc.op("pe", lambda e: e.matmul(p[:, 0:256], lhsT=Wkv[:, c, h * 128:(h + 1) * 128], rhs=hmT[:, c, :], start=(c == 0), stop=(c == 15)),
                          reads=[Wkv, hmT], writes=[p])
                sc.op("dve", lambda e: e.tensor_copy(out=KT[:, h, :], in_=p[:, 0:256]), reads=[p], writes=[KT])
            for half in range(2):
                p = pb[half]
                for c in range(16):
                    sc.op("pe", lambda e: e.matmul(p[:, :], lhsT=hmT[:, c, half * 128:(half + 1) * 128], rhs=Wkv[:, c, 512:1024], start=(c == 0), stop=(c == 15)),
                          reads=[Wkv, hmT], writes=[p])
                sc.op("dve", lambda e: e.tensor_copy(out=Vm[:, half, :], in_=p[:, :]), reads=[p], writes=[Vm])
            sc.barrier()
        sc.stack = st
        mT = sc.sb("mT", [128, 16, 512], BF16)
        xt = sc.sb("xt2", [128, 4, 2048], F32)
        hxT = sc.sb("hxT", [128, 16, 512], BF16)
        qT = sc.sb("qTc", [128, 4, 512], BF16)
        PT = [sc.sb(f"PTc{i}", [128, 512], BF16) for i in range(2)]
        rl = sc.sb("rlc", [128, 512], F32)
        oTn = sc.sb("oTn", [128, 4, 512], BF16)
        hmb = [sc.sb(f"hmb{i}", [128, 2048], BF16) for i in range(2)]
        afs = sc.sb("afs", [128, 4, 16], F32)
        sm = sc.sb("smx", [128, 4], F32)
        ex = sc.sb("ex", [128, 16], F32)
        ev = 0
        for blk in range(NB):
            sc.dma("sp", mT[:, :, :], mixT.rearrange("(c p) t -> p c t", p=128)[:, :, blk * 512:(blk + 1) * 512], writes=[mT])
            sc.dma("act", xt[:, :, :], x[blk * 512:(blk + 1) * 512, :].rearrange("(j p) d -> p j d", p=128), writes=[xt])
            for j in range(4):
                for cb in range(4):
                    p = pb[ev % 3]; ev += 1
                    for c in range(16):
                        sc.op("pe", lambda e: e.matmul(p[:, :], lhsT=mT[:, c, j * 128:(j + 1) * 128], rhs=Wo[:, c, cb * 512:(cb + 1) * 512],
                                                       start=(c == 0), stop=(c == 15)), reads=[mT, Wo], writes=[p])
                    sc.op("dve", lambda e: e.tensor_tensor(out=xt[:, j, cb * 512:(cb + 1) * 512], in0=p[:, :], in1=xt[:, j, cb * 512:(cb + 1) * 512], op=ALU.add),
                          reads=[p, xt], writes=[xt])
            for j in range(4):
                norm_T(sc, xt, xt[:, j, :], xn, ssb, rsb, junk, pT, idb, hxT, j)
            for h in range(4):
                p = pb[ev % 3]; ev += 1
                for c in range(16):
                    sc.op("pe", lambda e: e.matmul(p[:, :], lhsT=Wq[:, c, h * 128:(h + 1) * 128], rhs=hxT[:, c, :], start=(c == 0), stop=(c == 15)),
                          reads=[Wq, hxT], writes=[p])
                sc.op("act", lambda e: e.activation(out=qT[:, h, :], in_=p[:, :], func=AF.Copy), reads=[p], writes=[qT])
            for h in range(4):
                for half in range(2):
                    p = pb[ev % 3]; ev += 1
                    sc.op("pe", lambda e: e.matmul(p[:, :], lhsT=KT[:, h, half * 128:(half + 1) * 128], rhs=qT[:, h, :], start=True, stop=True),
                          reads=[KT, qT], writes=[p])
                    sc.op("act", lambda e: e.activation(out=PT[half][:, :], in_=p[:, :], func=AF.Exp, scale=128 ** -0.5), reads=[p], writes=[PT[half]])
                p = pb[ev % 3]; ev += 1
                for half in range(2):
                    sc.op("pe", lambda e: e.matmul(p[:, :], lhsT=Vm[:, half, h * 128:(h + 1) * 128], rhs=PT[half][:, :], start=(half == 0), stop=(half == 1)),
                          reads=[Vm, PT[half]], writes=[p])
                for half in range(2):
                    sc.op("pe", lambda e: e.matmul(psm[:, :], lhsT=onesb[:, :], rhs=PT[half][:, :], start=(half == 0), stop=(half == 1)),
                          reads=[onesb, PT[half]], writes=[psm])
                sc.op("dve", lambda e: e.reciprocal(out=rl[:, :], in_=psm[:, :]), reads=[psm], writes=[rl])
                sc.op("dve", lambda e: e.tensor_tensor(out=oTn[:, h, :], in0=p[:, :], in1=rl[:, :], op=ALU.mult), reads=[p, rl], writes=[oTn])
            for j in range(4):
                for cb in range(4):
                    p = pb[ev % 3]; ev += 1
                    for h in range(4):
                        sc.op("pe", lambda e: e.matmul(p[:, :], lhsT=oTn[:, h, j * 128:(j + 1) * 128], rhs=Wco[:, h, cb * 512:(cb + 1) * 512],
                                                       start=(h == 0), stop=(h == 3)), reads=[oTn, Wco], writes=[p])
                    sc.op("dve", lambda e: e.tensor_tensor(out=xt[:, j, cb * 512:(cb + 1) * 512], in0=p[:, :], in1=xt[:, j, cb * 512:(cb + 1) * 512], op=ALU.add),
                          reads=[p, xt], writes=[xt])
            sc.dma("sp", x2o[blk * 512:(blk + 1) * 512, :].rearrange("(j p) d -> p j d", p=128), xt[:, :, :], reads=[xt], writes=[o_x2], semb=xt)
            for j in range(4):
                hb = hmb[j % 2]
                norm_T(sc, xt, xt[:, j, :], hb, ssb, rsb, junk, pT, idb, hxT, j)
                t0 = blk * 512 + j * 128
                sc.dma("pool", hmo[t0:t0 + 128, :], hb[:, :], reads=[hb], writes=[o_hm], semb=hb)
            for j in range(4):
                for c in range(16):
                    sc.op("pe", lambda e: e.matmul(pl[:, :], lhsT=hxT[:, c, j * 128:(j + 1) * 128], rhs=Wr[:, c, :], start=(c == 0), stop=(c == 15)),
                          reads=[hxT, Wr], writes=[pl])
                sc.op("dve", lambda e: e.tensor_reduce(out=sm[:, 0:1], in_=pl[:, :], axis=AX.X, op=ALU.max), reads=[pl], writes=[sm])
                sc.op("dve", lambda e: e.tensor_scalar(out=sm[:, 1:2], in0=sm[:, 0:1], scalar1=-1.0, scalar2=None, op0=ALU.mult), reads=[sm], writes=[sm])
                sc.op("act", lambda e: e.activation(out=ex[:, :], in_=pl[:, :], func=AF.Exp, bias=sm[:, 1:2], scale=1.0, accum_out=sm[:, 2:3]),
                      reads=[pl, sm], writes=[ex, sm])
                sc.op("dve", lambda e: e.reciprocal(out=sm[:, 3:4], in_=sm[:, 2:3]), reads=[sm], writes=[sm])
                sc.op("dve", lambda e: e.tensor_scalar(out=afs[:, j, :], in0=ex[:, :], scalar1=sm[:, 3:4], scalar2=None, op0=ALU.mult), reads=[ex, sm], writes=[afs])
            sc.dma("sp", affo[blk * 512:(blk + 1) * 512, :].rearrange("(j p) e -> p j e", p=128), afs[:, :, :], reads=[afs], writes=[o_aff], semb=afs)
        sc.barrier()
        sc.stack = old


def moe_phase(nc, sc, S, hm, affc, wg, wu, wd, gmoe, su_d, wbf, xs, yd, part, idb, id32, ones32, NE=4, dbg_slot=None):
    F = S // 128
    CAP = S // 8
    NSB = max(CAP // 512, 1)
    SBW = min(CAP, 512)
    b_wbf, b_xs, b_y, b_part = Buf(wbf, "wbf"), Buf(xs[0], "xs"), Buf(yd[0], "yd"), Buf(part, "part")
    with ExitStack() as st:
        sc.stack, old = st, sc.stack
        su = sc.sb("su", [128, 128], F32)
        sc.dma("sp", su[:, :], su_d[:, :], writes=[su])
        gm = sc.sb("gm3", [128, 16], F32)
        sc.dma("sp", gm[:, :], gmoe[:, :], writes=[gm])
        aff = sc.sb("aff", [128, NE, F], F32)
        sc.dma("sp", aff[:, :, :], affc.rearrange("e p f -> p e f"), writes=[aff])
        slot = sc.sb("slot", [128, NE * F], I32)
        with ExitStack() as st2:
            sc.stack = st2
            lo = sc.sb("lo", [128, 1], F32)
            mid = sc.sb("mid", [128, 1], F32)
            cnt = sc.sb("cnt", [128, 1], F32)
            cf = sc.sb("cf", [128, 1], F32)
            msk = sc.sb("msk", [128, F], F32)
            R = sc.sb("R", [128, F], F32)
            mT = sc.sb("mT", [128, 128], F32)
            tmp = sc.sb("tmp", [128, F], F32)
            pt = sc.ps("pt", [128, 8], F32)
            pp = sc.ps("pp", [128, 128], F32)
            pq = sc.ps("pq", [128, 128], F32)
            for e in range(NE):
                a = aff[:, e, :]
                sc.op("dve", lambda en: en.memset(lo[:, :], 0.0), writes=[lo])
                for i in range(30):
                    w = 2.0 ** -(i + 1)
                    sc.op("dve", lambda en: en.tensor_scalar(out=mid[:, :], in0=lo[:, :], scalar1=w, scalar2=None, op0=ALU.add), reads=[lo], writes=[mid])
                    sc.op("dve", lambda en: en.tensor_scalar(out=msk[:, :], in0=a, scalar1=mid[:, 0:1], scalar2=None, op0=ALU.is_ge), reads=[aff, mid], writes=[msk])
                    sc.op("dve", lambda en: en.reduce_sum(out=cnt[:, :], in_=msk[:, :], axis=AX.X), reads=[msk], writes=[cnt])
                    sc.op("pe", lambda en: en.matmul(pt[:, 0:1], lhsT=ones32[:, :], rhs=cnt[:, :], start=True, stop=True), reads=[ones32, cnt], writes=[pt])
                    sc.op("dve", lambda en: en.tensor_scalar(out=cf[:, :], in0=pt[:, 0:1], scalar1=CAP - 0.5, scalar2=None, op0=ALU.is_ge), reads=[pt], writes=[cf])
                    sc.op("dve", lambda en: en.scalar_tensor_tensor(out=lo[:, :], in0=cf[:, :], scalar=w, in1=lo[:, :], op0=ALU.mult, op1=ALU.add),
                          reads=[cf, lo], writes=[lo])
                sc.op("dve", lambda en: en.tensor_scalar(out=msk[:, :], in0=a, scalar1=lo[:, 0:1], scalar2=None, op0=ALU.is_ge), reads=[aff, lo], writes=[msk])
                sc.op("dve", lambda en: en.reduce_sum(out=cnt[:, :], in_=msk[:, :], axis=AX.X), reads=[msk], writes=[cnt])
                sc.op("dve", lambda en: en.tensor_scalar(out=R[:, :], in0=ones32[:, 0:F], scalar1=cnt[:, 0:1], scalar2=None, op0=ALU.mult), reads=[ones32, cnt], writes=[R])
                sc.op("pe", lambda en: en.transpose(out=pq[0:F, :], in_=msk[:, :], identity=id32[:, :]), reads=[msk, id32], writes=[pq])
                sc.op("dve", lambda en: en.tensor_copy(out=mT[0:F, :], in_=pq[0:F, :]), reads=[pq], writes=[mT])
                sc.op("pe", lambda en: en.matmul(pp[:, 0:F], lhsT=su[:, :], rhs=R[:, :], start=True, stop=False), reads=[su, R], writes=[pp])
                sc.op("pe", lambda en: en.matmul(pp[:, 0:F], lhsT=mT[0:F, :], rhs=su[0:F, 0:F], start=False, stop=True), reads=[mT, su], writes=[pp])
                sc.op("dve", lambda en: en.tensor_scalar(out=tmp[:, :], in0=msk[:, :], scalar1=-1.0e6, scalar2=1.0e6, op0=ALU.mult, op1=ALU.add), reads=[msk], writes=[tmp])
                sc.op("dve", lambda en: en.tensor_tensor(out=tmp[:, :], in0=pp[:, 0:F], in1=tmp[:, :], op=ALU.add), reads=[pp, tmp], writes=[tmp])
                sc.op("dve", lambda en: en.tensor_copy(out=slot[:, e * F:(e + 1) * F], in_=tmp[:, :]), reads=[tmp], writes=[slot])
            if dbg_slot is not None:
                sc.dma("sp", dbg_slot[:, :], slot[:, :], reads=[slot], writes=[Buf(dbg_slot, "dbgs")], semb=slot)
            sc.barrier()
        with ExitStack() as st3:
            sc.stack = st3
            hr = [sc.sb(f"hr{i}", [128, 2048], BF16) for i in range(3)]
            hv = hm.rearrange("(p f) d -> f p d", f=F)
            for f in range(F):
                h = hr[f % 3]
                sc.dma("sp" if f % 2 == 0 else "act", h[:, :], hv[f], writes=[h])
                for e in range(NE):
                    sc.idma(xs[e][:, :], slot[:, e * F + f:e * F + f + 1], h[:, :], None, CAP - 1, reads=[h, slot], writes=[b_xs], semb=h)
            sc.barrier()
        with ExitStack() as st4:
            sc.stack = st4
            stg = [sc.sb(f"stg3{i}", [128, 2048], F32) for i in range(2)]
            wcb = [sc.sb(f"wcb{i}", [128, 2048], BF16) for i in range(2)]
            Wd = sc.sb("Wd", [128, 16, 2048], BF16)
            xsT = sc.sb("xsT", [128, 16, SBW], BF16)
            hT = sc.sb("hT3", [128, 16, SBW], BF16)
            Wgc = [sc.sb(f"Wgc{i}", [128, 16, 128], BF16) for i in range(2)]
            Wuc = [sc.sb(f"Wuc{i}", [128, 16, 128], BF16) for i in range(2)]
            xst = [sc.sb(f"xst{i}", [128, 2048], BF16) for i in range(2)]
            sg = sc.sb("sg", [128, SBW], F32)
            yst = [sc.sb(f"yst{i}", [128, 2048], BF16) for i in range(2)]
            pT = [sc.ps(f"pT3{i}", [128, 1024], BF16) for i in range(2)]
            pa = [sc.ps(f"pa{i}", [128, 512], F32) for i in range(2)]
            pu = [sc.ps(f"pu{i}", [128, 512], F32) for i in range(2)]
            py = [sc.ps(f"py{i}", [128, 512], F32) for i in range(2)]
            k = 0
            ev = 0
            for e in range(NE):
                for wi, wsrc in enumerate((wg, wu, wd)):
                    for c in range(16):
                        b = stg[k % 2]; o = wcb[k % 2]; k += 1
                        sc.dma("sp" if k % 2 else "act", b[:, :], wsrc[e, c * 128:(c + 1) * 128, :], writes=[b])
                        if wi < 2:
                            sc.op("dve", lambda en: en.tensor_scalar(out=o[:, :], in0=b[:, :], scalar1=gm[:, c:c + 1], scalar2=None, op0=ALU.mult),
                                  reads=[b, gm], writes=[o])
                            sc.dma("pool", wbf[wi, c * 128:(c + 1) * 128, :], o[:, :], reads=[o], writes=[b_wbf], semb=o)
                        else:
                            sc.op("dve", lambda en: en.tensor_copy(out=Wd[:, c, :], in_=b[:, :]), reads=[b], writes=[Wd])
                sc.barrier()
                for sb in range(NSB):
                    for j in range(SBW // 128):
                        xt = xst[j % 2]
                        r0 = sb * SBW + j * 128
                        sc.dma("sp", xt[:, :], xs[e][r0:r0 + 128, :], reads=[b_xs], writes=[xt])
                        for half in range(2):
                            p = pT[half]
                            for c8 in range(8):
                                c = half * 8 + c8
                                sc.op("pe", lambda en: en.transpose(out=p[:, c8 * 128:(c8 + 1) * 128], in_=xt[:, c * 128:(c + 1) * 128], identity=idb[:, :]),
                                      reads=[xt, idb], writes=[p])
                            dst = xsT[:, half * 8:(half + 1) * 8, j * 128:(j + 1) * 128]
                            src = p[:, :].rearrange("p (c t) -> p c t", c=8)
                            if half == 0:
                                sc.op("dve", lambda en: en.tensor_copy(out=dst, in_=src), reads=[p], writes=[xsT])
                            else:
                                sc.op("act", lambda en: en.activation(out=dst, in_=src, func=AF.Copy), reads=[p], writes=[xsT])
                    for fc in range(16):
                        g_, u_ = Wgc[fc % 2], Wuc[fc % 2]
                        sc.dma("sp", g_[:, :, :], wbf[0].rearrange("(c p) f -> p c f", p=128)[:, :, fc * 128:(fc + 1) * 128], reads=[b_wbf], writes=[g_])
                        sc.dma("act", u_[:, :, :], wbf[1].rearrange("(c p) f -> p c f", p=128)[:, :, fc * 128:(fc + 1) * 128], reads=[b_wbf], writes=[u_])
                        A, U = pa[fc % 2], pu[fc % 2]
                        for c in range(16):
                            sc.op("pe", lambda en: en.matmul(A[:, 0:SBW], lhsT=g_[:, c, :], rhs=xsT[:, c, :], start=(c == 0), stop=(c == 15)), reads=[g_, xsT], writes=[A])
                        for c in range(16):
                            sc.op("pe", lambda en: en.matmul(U[:, 0:SBW], lhsT=u_[:, c, :], rhs=xsT[:, c, :], start=(c == 0), stop=(c == 15)), reads=[u_, xsT], writes=[U])
                        sc.op("act", lambda en: en.activation(out=sg[:, :], in_=A[:, 0:SBW], func=AF.Silu), reads=[A], writes=[sg])
                        sc.op("dve", lambda en: en.tensor_tensor(out=hT[:, fc, :], in0=U[:, 0:SBW], in1=sg[:, :], op=ALU.mult), reads=[U, sg], writes=[hT])
                    for j in range(SBW // 128):
                        ys = yst[j % 2]
                        for cb in range(4):
                            Y = py[ev % 2]; ev += 1
                            for fc in range(16):
                                sc.op("pe", lambda en: en.matmul(Y[:, :], lhsT=hT[:, fc, j * 128:(j + 1) * 128], rhs=Wd[:, fc, cb * 512:(cb + 1) * 512],
                                                                 start=(fc == 0), stop=(fc == 15)), reads=[hT, Wd], writes=[Y])
                            if cb % 2 == 0:
                                sc.op("dve", lambda en: en.tensor_copy(out=ys[:, cb * 512:(cb + 1) * 512], in_=Y[:, :]), reads=[Y], writes=[ys])
                            else:
                                sc.op("act", lambda en: en.activation(out=ys[:, cb * 512:(cb + 1) * 512], in_=Y[:, :], func=AF.Copy), reads=[Y], writes=[ys])
                        r0 = sb * SBW + j * 128
                        sc.dma("pool", yd[e][r0:r0 + 128, :], ys[:, :], reads=[ys], writes=[b_y], semb=ys)
                sc.barrier()
        with ExitStack() as st5:
            sc.stack = st5
            gt = [[sc.sb(f"gt{i}{e}", [128, 2048], BF16) for e in range(NE)] for i in range(2)]
            acc = sc.sb("acc3", [128, 2048], F32)
            ob = [sc.sb(f"ob{i}", [128, 2048], BF16) for i in range(2)]
            pv = part.rearrange("(p f) d -> f p d", f=F)
            for f in range(F):
                for e in range(NE):
                    t = gt[f % 2][e]
                    sc.op("pool" if e % 2 == 0 else "dve", lambda en: en.memset(t[:, :], 0.0), writes=[t])
                    sc.idma(t[:, :], None, yd[e][:, :], slot[:, e * F + f:e * F + f + 1], CAP - 1, reads=[b_y, slot], writes=[t], semb=t)
                for e in range(NE):
                    t = gt[f % 2][e]
                    if e == 0:
                        sc.op("dve", lambda en: en.tensor_scalar(out=acc[:, :], in0=t[:, :], scalar1=aff[:, e, f:f + 1], scalar2=None, op0=ALU.mult),
                              reads=[t, aff], writes=[acc])
                    elif e < NE - 1:
                        sc.op("dve", lambda en: en.scalar_tensor_tensor(out=acc[:, :], in0=t[:, :], scalar=aff[:, e, f:f + 1], in1=acc[:, :],
                                                                        op0=ALU.mult, op1=ALU.add), reads=[t, aff, acc], writes=[acc])
                    else:
                        o = ob[f % 2]
                        sc.op("dve", lambda en: en.scalar_tensor_tensor(out=o[:, :], in0=t[:, :], scalar=aff[:, e, f:f + 1], in1=acc[:, :],
                                                                        op0=ALU.mult, op1=ALU.add), reads=[t, aff, acc], writes=[o])
                        sc.dma("sp", pv[f], o[:, :], reads=[o], writes=[b_part], semb=o)
            sc.barrier()
        sc.stack = old


def final_phase(nc, sc, T, x2, parts, gfin, out, NP=4):
    b_out = Buf(out, "outf")
    with ExitStack() as st:
        sc.stack, old = st, sc.stack
        g = sc.sb("gf", [128, 2048], F32)
        sc.dma("sp", g[:, :], gfin[:, :], writes=[g])
        xt = [sc.sb(f"xf{i}", [128, 2048], F32) for i in range(2)]
        pt = [sc.sb(f"pf{i}", [128, max(NP, 1), 2048], BF16) for i in range(2)]
        junk = sc.sb("junkf", [128, 2048], BF16)
        ss = sc.sb("ssf", [128, 1], F32)
        rs = sc.sb("rsf", [128, 1], F32)
        ot = [sc.sb(f"of{i}", [128, 2048], F32) for i in range(2)]
        for t in range(T // 128):
            x_, p_, o_ = xt[t % 2], pt[t % 2], ot[t % 2]
            sc.dma("sp", x_[:, :], x2[t * 128:(t + 1) * 128, :], writes=[x_])
            if NP > 0:
                sc.dma("act", p_[:, :, :], parts[:, t * 128:(t + 1) * 128, :].rearrange("n p d -> p n d"), writes=[p_])
            for n in range(NP):
                sc.op("dve" if n % 2 == 0 else "pool", lambda en: en.tensor_tensor(out=x_[:, :], in0=x_[:, :], in1=p_[:, n, :], op=ALU.add), reads=[x_, p_], writes=[x_])
            sc.op("act", lambda en: en.activation(out=junk[:, :], in_=x_[:, :], func=AF.Square, accum_out=ss[:, :]), reads=[x_], writes=[junk, ss])
            rsqrt(sc, rs, rs[:, :], ss, ss[:, :], 1.0 / 2048)
            sc.op("dve", lambda en: en.scalar_tensor_tensor(out=o_[:, :], in0=x_[:, :], scalar=rs[:, 0:1], in1=g[:, :], op0=ALU.mult, op1=ALU.mult),
                  reads=[x_, rs, g], writes=[o_])
            sc.dma("pool", out[t * 128:(t + 1) * 128, :], o_[:, :], reads=[o_], writes=[b_out], semb=o_)
        sc.barrier()
        sc.stack = old

def t5_bucket_np(rel):
    n = np.abs(rel)
    with np.errstate(divide='ignore'):
        large = 8 + (np.log(np.maximum(n, 1).astype(np.float32) / 8) / np.log(128 / 8) * 8).astype(np.int32)
    large = np.minimum(large, 15)
    return np.where(rel > 0, 16, 0) + np.where(n < 8, n, large)

def w_cols(g):
    cols = []
    def rng(base, h, width): return list(range(base + h * width, base + (h + 1) * width))
    hs = [2 * g, 2 * g + 1]
    for h in hs: cols += rng(0, h, 128)
    for h in hs: cols += rng(1024, h, 128)
    for h in hs: cols += rng(3072, h, 128)
    for h in hs: cols += rng(4096, h, 128)
    for h in hs: cols += rng(5120, h, 128)
    for h in hs: cols += rng(2048, h, 128)
    for h in hs: cols += rng(6144, h, 128)
    for base in (7168, 7176, 7184, 7192):
        for h in hs: cols.append(base + h)
    return np.array(cols)

def bias6(table, h):
    k = np.arange(128)[:, None, None]; r = np.arange(6)[None, :, None]; q = np.arange(512)[None, None, :]
    rel = (r - 1) * 128 + k - q
    return np.ascontiguousarray(table[t5_bucket_np(rel), h]).astype(np.float32)

def gdn_consts():
    j = np.arange(128)[:, None]; i = np.arange(128)[None, :]
    cm = np.zeros((10, 128, 128), np.float32)
    cm[0] = (j <= i); cm[1] = (j >= i)
    cm[2] = (j == 127); cm[3] = (j == 0)
    cm[4] = np.where(i >= j, 0.0, -30000.0); cm[5] = np.where(i <= j, 0.0, -30000.0)
    cm[6] = (i > j); cm[7] = (i < j)
    cm[8] = 1.0
    return cm

def conv_cols(g):
    cols = []
    for base in (0, 1024, 2048):
        for h in (2 * g, 2 * g + 1):
            cols.append(np.arange(base + h * 128, base + (h + 1) * 128))
    return cols


S_FULL = 16384
T_TOK = 4096


def _common(nc, sc, idn):
    id32 = sc.sb("id32", [128, 128], F32)
    idb = sc.sb("idb", [128, 128], BF16)
    ones32 = sc.sb("ones32", [128, 128], F32)
    onesb = sc.sb("onesbb", [128, 128], BF16)
    sc.dma("sp", id32[:, :], idn[:, :], writes=[id32])
    sc.op("dve", lambda e: e.tensor_copy(out=idb[:, :], in_=id32[:, :]), reads=[id32], writes=[idb])
    sc.op("dve", lambda e: e.memset(ones32[:, :], 1.0), writes=[ones32])
    sc.op("dve", lambda e: e.memset(onesb[:, :], 1.0), writes=[onesb])
    init_consts(sc)
    return id32, idb, ones32, onesb


def build_mixer(S):
    nc = bass.Bass("TRN2", target_bir_lowering=False)
    dt_ = lambda n, s, d=F32, k="ExternalInput": nc.dram_tensor(n, s, d, kind=k).ap()
    x = dt_("x", [S, 2048]); w = dt_("w", [2048, 1800]); gmix = dt_("gmix", [128, 16])
    b6 = dt_("b6", [2, 128, 6, 512]); cst = dt_("cst", [2, 128, 2]); lamv = dt_("lamv", [128, 4, 64]); sub = dt_("sub", [128, 1])
    cw = dt_("cw", [6, 128, 5]); gpar = dt_("gpar", [2, 128, 4]); gnorm = dt_("gnorm", [128, 128]); cmd = dt_("cmd", [10, 128, 128])
    idn = dt_("idn", [128, 128])
    fT = dt_("fT", [10, 128, S], BF16, "Internal"); tm = dt_("tm", [S, 512], BF16, "Internal"); gates = dt_("gates", [S, 8], F32, "Internal")
    mix = dt_("mix", [S, 512], BF16, "ExternalOutput")
    with ExitStack() as st:
        sc = Sched(nc, st)
        set_dram_bufs(Buf(fT, "fT"), Buf(tm, "tm"), Buf(gates, "gates"))
        set_mix_buf(Buf(mix, "mix"))
        id32, idb, ones32, onesb = _common(nc, sc, idn)
        proj_phase(nc, sc, S, x, w, gmix, fT, tm, gates, idb)
        da_phase(nc, sc, S, fT, tm, mix, b6, cst, lamv, sub, idb, ones32)
        gdn_phase(nc, sc, S, fT, tm, gates, mix, cw, gpar, gnorm, cmd, idb, id32, ones32)
        sc.barrier()
    return nc


def build_tok(T):
    nc = bass.Bass("TRN2", target_bir_lowering=False)
    dt_ = lambda n, s, d=F32, k="ExternalInput": nc.dram_tensor(n, s, d, kind=k).ap()
    x = dt_("x", [T, 2048]); mixT = dt_("mixT", [2048, T], BF16); mem = dt_("mem", [256, 2048])
    w_out = dt_("w_out", [2048, 2048]); w_cq = dt_("w_cq", [2048, 512]); w_ckv = dt_("w_ckv", [2048, 1024]); w_co = dt_("w_co", [512, 2048])
    w_r = dt_("w_r", [2048, 16]); gains = dt_("gains", [3, 128, 16]); idn = dt_("idn", [128, 128])
    x2o = dt_("x2o", [T, 2048], F32, "ExternalOutput"); hmo = dt_("hmo", [T, 2048], BF16, "ExternalOutput"); affo = dt_("affo", [T, 16], F32, "ExternalOutput")
    with ExitStack() as st:
        sc = Sched(nc, st)
        id32, idb, ones32, onesb = _common(nc, sc, idn)
        tok_phase(nc, sc, T, x, mixT, mem, w_out, w_cq, w_ckv, w_co, w_r, gains, x2o, hmo, affo, idb, onesb)
        sc.barrier()
    return nc


def build_moe(S, NE=4):
    nc = bass.Bass("TRN2", target_bir_lowering=False)
    dt_ = lambda n, s, d=F32, k="ExternalInput": nc.dram_tensor(n, s, d, kind=k).ap()
    F = S // 128
    CAP = S // 8
    hm = dt_("hm", [S, 2048], BF16); affc = dt_("affc", [NE, 128, F])
    wg = dt_("wg", [NE, 2048, 2048]); wu = dt_("wu", [NE, 2048, 2048]); wd = dt_("wd", [NE, 2048, 2048])
    gmoe = dt_("gmoe", [128, 16]); sud = dt_("sud", [128, 128]); idn = dt_("idn", [128, 128])
    wbf = dt_("wbf", [2, 2048, 2048], BF16, "Internal")
    xs = [dt_(f"xs{e}", [CAP, 2048], BF16, "Internal") for e in range(NE)]
    yd = [dt_(f"yd{e}", [CAP, 2048], BF16, "Internal") for e in range(NE)]
    part = dt_("part", [S, 2048], BF16, "ExternalOutput")
    with ExitStack() as st:
        sc = Sched(nc, st)
        id32, idb, ones32, onesb = _common(nc, sc, idn)
        moe_phase(nc, sc, S, hm, affc, wg, wu, wd, gmoe, sud, wbf, xs, yd, part, idb, id32, ones32, NE=NE)
        sc.barrier()
    return nc


def build_final(T, NP):
    nc = bass.Bass("TRN2", target_bir_lowering=False)
    dt_ = lambda n, s, d=F32, k="ExternalInput": nc.dram_tensor(n, s, d, kind=k).ap()
    x2 = dt_("x2", [T, 2048]); gfin = dt_("gfin", [128, 2048])
    parts = dt_("parts", [max(NP, 1), T, 2048], BF16)
    out = dt_("out", [T, 2048], F32, "ExternalOutput")
    with ExitStack() as st:
        sc = Sched(nc, st)
        init_consts(sc)
        final_phase(nc, sc, T, x2, parts, gfin, out, NP=NP)
        sc.barrier()
    return nc


def kernel(x, mem, rel_bias_table, norm_mix, w_in, conv_w, lambda_q1, lambda_k1, lambda_q2, lambda_k2, da_subln, gdn_a_log, gdn_dt_bias,
           gdn_norm, w_out, norm_cross, norm_mem, w_cq, w_ckv, w_co, norm_moe, w_router, w_gate, w_up, w_down, norm_final):
    f32 = np.float32
    A = lambda a: np.ascontiguousarray(np.asarray(a))
    x = np.asarray(x); S = x.shape[1]
    eye = np.eye(128, dtype=f32)
    tab = np.asarray(rel_bias_table)
    nc1 = build_mixer(S)
    lamv = np.broadcast_to(np.stack([np.asarray(lambda_q1)[0], np.asarray(lambda_k1)[0], np.asarray(lambda_q2)[0], np.asarray(lambda_k2)[0]])[None],
                           (128, 4, 64)).astype(f32).copy()
    cmd = gdn_consts()
    in1 = []
    for c in range(8):
        b, g = c // 4, c % 4
        gp = np.zeros((2, 128, 4), f32)
        for hg in range(2):
            h = 2 * g + hg
            gp[hg, :, 0] = np.asarray(gdn_a_log)[0, 0, h]; gp[hg, :, 1] = np.asarray(gdn_dt_bias)[0, 0, h]
            gp[hg, :, 2] = np.asarray(gdn_a_log)[0, 1, h]; gp[hg, :, 3] = np.asarray(gdn_dt_bias)[0, 1, h]
        in1.append(dict(
            x=A(x[b]), w=A(np.asarray(w_in)[0][:, w_cols(g)]), gmix=A(np.asarray(norm_mix)[0].reshape(16, 128).T),
            b6=np.stack([bias6(tab, 2 * g + i) for i in range(2)]),
            cst=np.stack([np.broadcast_to(tab[[15, 31], 2 * g + i][None, :], (128, 2)) for i in range(2)]).astype(f32).copy(),
            lamv=lamv, sub=A(np.asarray(da_subln)[0].reshape(128, 1)),
            cw=np.stack([A(np.asarray(conv_w)[0][:, cc].T) for cc in conv_cols(g)]), gpar=gp,
            gnorm=np.broadcast_to(np.asarray(gdn_norm)[0][None, :], (128, 128)).astype(f32).copy(), cmd=cmd, idn=eye))
    r1 = run_bass_kernel_spmd(nc1, in1, core_ids=list(range(8))).results
    mixfull = np.zeros((2, S, 2048), dtype=ml_dtypes.bfloat16)
    for c in range(8):
        b, g = c // 4, c % 4
        m = np.asarray(r1[c]["mix"])
        mixfull[b, :, 2 * g * 128:(2 * g + 2) * 128] = m[:, 0:256]
        mixfull[b, :, 1024 + 2 * g * 128:1024 + (2 * g + 2) * 128] = m[:, 256:512]
    T = S // 4
    nc2 = build_tok(T)
    g3 = np.stack([np.asarray(k)[0].reshape(16, 128).T for k in (norm_cross, norm_mem, norm_moe)]).astype(f32)
    in2 = []
    for c in range(8):
        b, s = c // 4, c % 4
        in2.append(dict(x=A(x[b, s * T:(s + 1) * T]), mixT=A(mixfull[b, s * T:(s + 1) * T].T), mem=A(np.asarray(mem)[b]),
                        w_out=A(np.asarray(w_out)[0]), w_cq=A(np.asarray(w_cq)[0]), w_ckv=A(np.asarray(w_ckv)[0]), w_co=A(np.asarray(w_co)[0]),
                        w_r=A(np.asarray(w_router)[0]), gains=A(g3), idn=eye))
    r2 = run_bass_kernel_spmd(nc2, in2, core_ids=list(range(8))).results
    nc3 = build_moe(S)
    hm_full = [np.concatenate([np.asarray(r2[b * 4 + s]["hmo"]) for s in range(4)], axis=0) for b in range(2)]
    aff_full = [np.concatenate([np.asarray(r2[b * 4 + s]["affo"]) for s in range(4)], axis=0) for b in range(2)]
    su = np.triu(np.ones((128, 128), f32), 1)
    gmoe = A(np.asarray(norm_moe)[0].reshape(16, 128).T)
    in3 = []
    for c in range(8):
        b, eq = c // 4, c % 4
        es = slice(4 * eq, 4 * eq + 4)
        in3.append(dict(hm=A(hm_full[b]), affc=A(aff_full[b][:, es].T.reshape(4, 128, S // 128)),
                        wg=A(np.asarray(w_gate)[0][es]), wu=A(np.asarray(w_up)[0][es]), wd=A(np.asarray(w_down)[0][es]),
                        gmoe=gmoe, sud=su, idn=eye))
    r3 = run_bass_kernel_spmd(nc3, in3, core_ids=list(range(8))).results
    nc4 = build_final(T, 4)
    gfin = np.broadcast_to(np.asarray(norm_final)[None, :], (128, 2048)).astype(f32).copy()
    in4 = []
    for c in range(8):
        b, s = c // 4, c % 4
        parts = np.stack([np.asarray(r3[b * 4 + eq]["part"])[s * T:(s + 1) * T] for eq in range(4)])
        in4.append(dict(x2=A(r2[c]["x2o"]), gfin=gfin, parts=A(parts)))
    r3 = run_bass_kernel_spmd(nc4, in4, core_ids=list(range(8))).results
    out = np.zeros((2, S, 2048), f32)
    for c in range(8):
        b, s = c // 4, c % 4
        out[b, s * T:(s + 1) * T] = np.asarray(r3[c]["out"])
    return out
```
